# Optimizing a Trainium2 kernel written in Bass

```python
import math
import jax, jax.numpy as jnp
from jax import lax
import numpy as np

D_MODEL = 2048
BATCH = 4
SEQ = 2048
DEPTH = 2

N_MIXERS = 2
N_SB_LAYERS = (DEPTH + 1) // 2
N_GDN_LAYERS = DEPTH // 2

SB_HEADS = 16
SB_HEAD_DIM = D_MODEL // SB_HEADS
SB_WIDTH = SB_HEADS * SB_HEAD_DIM
SB_BLOCK = 128

GDN_K_HEADS = 16
GDN_V_HEADS = 32
GDN_HEAD_DIM = D_MODEL // 16
GDN_KEY_DIM = GDN_K_HEADS * GDN_HEAD_DIM
GDN_VALUE_DIM = GDN_V_HEADS * GDN_HEAD_DIM
GDN_CONV = 4
GDN_CONV_CH = 2 * GDN_KEY_DIM + GDN_VALUE_DIM
GDN_IN = GDN_CONV_CH + GDN_VALUE_DIM + 2 * GDN_V_HEADS
GDN_CHUNK = 64

N_EXPERTS = 32
TOP_K = 4
EXPERT_FF = D_MODEL
SWIGLU_LIMIT = 7.0
SWIGLU_ALPHA = 1.702
MOE_BLOCK = 128

DEEPNORM_ALPHA = (2 * DEPTH) ** 0.25
DEEPNORM_BETA = (8 * DEPTH) ** -0.25

LN_EPS = 1e-5
RMS_EPS = 1e-6
L2_EPS = 1e-6

kernel_name = 'hybrid_stickbreak_gdn_moe_deepnorm'


def layer_norm(x, g, b):
    xf = x.astype(jnp.float32)
    mu = jnp.mean(xf, axis=-1, keepdims=True)
    xc = xf - mu
    var = jnp.mean(xc * xc, axis=-1, keepdims=True)
    return (xc * lax.rsqrt(var + LN_EPS) * g.astype(jnp.float32) + b.astype(jnp.float32)).astype(x.dtype)


def l2norm(x):
    xf = x.astype(jnp.float32)
    return xf * lax.rsqrt(jnp.sum(xf * xf, axis=-1, keepdims=True) + L2_EPS)


def stick_breaking_attention(h, w_qkv, w_o):
    B, T, _ = h.shape
    qkv = (h @ w_qkv).reshape(B, T, 3, SB_HEADS, SB_HEAD_DIM)
    q = jnp.moveaxis(qkv[:, :, 0], 2, 1).astype(jnp.float32) * (SB_HEAD_DIM ** -0.5)
    k = jnp.moveaxis(qkv[:, :, 1], 2, 1).astype(jnp.float32)
    v = jnp.moveaxis(qkv[:, :, 2], 2, 1)
    outs = []
    for blk in range(T // SB_BLOCK):
        q0 = blk * SB_BLOCK
        n_keys = q0 + SB_BLOCK
        z = jnp.einsum('bhqd,bhkd->bhqk', q[:, :, q0:n_keys], k[:, :, :n_keys])
        causal = jnp.arange(n_keys)[None, :] < (q0 + jnp.arange(SB_BLOCK))[:, None]
        log_1m_beta = jnp.where(causal, jax.nn.log_sigmoid(-z), 0.0)
        log_stick = lax.cumsum(log_1m_beta, axis=3, reverse=True) - log_1m_beta
        weights = jnp.where(causal, jnp.exp(jax.nn.log_sigmoid(z) + log_stick), 0.0)
        outs.append(jnp.einsum('bhqk,bhkd->bhqd', weights.astype(v.dtype), v[:, :, :n_keys]))
    o = jnp.concatenate(outs, axis=2)
    return jnp.moveaxis(o, 1, 2).reshape(B, T, SB_WIDTH) @ w_o


def causal_depthwise_conv(x, w):
    kw, ch = w.shape
    return lax.conv_general_dilated(
        x, w[:, None, :].astype(x.dtype), window_strides=(1,), padding=[(kw - 1, 0)],
        dimension_numbers=('NWC', 'WIO', 'NWC'), feature_group_count=ch)


def chunk_gated_delta_rule(q, k, v, g, beta):
    B, T, H, DK = k.shape
    DV = v.shape[-1]
    C = GDN_CHUNK
    N = T // C

    def chunks(t):
        t = t.astype(jnp.float32).reshape((B, N, C, H) + t.shape[3:])
        return jnp.moveaxis(t, 3, 1)

    q_c, k_c, v_c = chunks(q), chunks(k), chunks(v)
    g_c = lax.cumsum(chunks(g), axis=3)
    b_c = chunks(beta)
    incl = jnp.tril(jnp.ones((C, C), dtype=bool))
    strict = jnp.tril(jnp.ones((C, C), dtype=bool), -1)
    diff = g_c[..., :, None] - g_c[..., None, :]
    decay = jnp.where(incl, jnp.exp(jnp.where(incl, diff, 0.0)), 0.0)
    k_beta = k_c * b_c[..., None]
    lower = jnp.where(strict, jnp.einsum('bhnid,bhnjd->bhnij', k_beta, k_c) * decay, 0.0)
    unit_lower = lower + jnp.eye(C, dtype=jnp.float32)
    u = lax.linalg.triangular_solve(unit_lower, v_c * b_c[..., None], left_side=True, lower=True, unit_diagonal=True)
    w = lax.linalg.triangular_solve(unit_lower, k_beta * jnp.exp(g_c)[..., None], left_side=True, lower=True, unit_diagonal=True)
    qk = jnp.where(incl, jnp.einsum('bhnid,bhnjd->bhnij', q_c, k_c) * decay, 0.0)
    xs = tuple(jnp.moveaxis(t, 2, 0) for t in (q_c, k_c, u, w, g_c, qk))

    def step(state, inp):
        q_i, k_i, u_i, w_i, g_i, qk_i = inp
        v_new = u_i - jnp.einsum('bhcd,bhde->bhce', w_i, state)
        o_i = (jnp.einsum('bhcd,bhde->bhce', q_i * jnp.exp(g_i)[..., None], state)
               + jnp.einsum('bhij,bhje->bhie', qk_i, v_new))
        g_last = g_i[..., -1:]
        state = (state * jnp.exp(g_last)[..., None]
                 + jnp.einsum('bhcd,bhce->bhde', k_i * jnp.exp(g_last - g_i)[..., None], v_new))
        return state, o_i

    state0 = jnp.zeros((B, H, DK, DV), jnp.float32)
    _, o = lax.scan(step, state0, xs)
    o = jnp.moveaxis(o, 0, 2)
    return jnp.moveaxis(o, 1, 3).reshape(B, T, H, DV)


def gated_deltanet(h, w_in, conv_w, a_log, dt_bias, norm_w, w_o):
    B, T, _ = h.shape
    proj = h @ w_in
    s1 = GDN_CONV_CH
    s2 = s1 + GDN_VALUE_DIM
    s3 = s2 + GDN_V_HEADS
    qkv, z, b, a = proj[..., :s1], proj[..., s1:s2], proj[..., s2:s3], proj[..., s3:]
    qkv = jax.nn.silu(causal_depthwise_conv(qkv, conv_w))
    q = qkv[..., :GDN_KEY_DIM].reshape(B, T, GDN_K_HEADS, GDN_HEAD_DIM)
    k = qkv[..., GDN_KEY_DIM:2 * GDN_KEY_DIM].reshape(B, T, GDN_K_HEADS, GDN_HEAD_DIM)
    v = qkv[..., 2 * GDN_KEY_DIM:].reshape(B, T, GDN_V_HEADS, GDN_HEAD_DIM)
    rep = GDN_V_HEADS // GDN_K_HEADS
    q = jnp.repeat(l2norm(q) * (GDN_HEAD_DIM ** -0.5), rep, axis=2)
    k = jnp.repeat(l2norm(k), rep, axis=2)
    beta = jax.nn.sigmoid(b.astype(jnp.float32))
    g = -jnp.exp(a_log.astype(jnp.float32)) * jax.nn.softplus(a.astype(jnp.float32) + dt_bias.astype(jnp.float32))
    o = chunk_gated_delta_rule(q, k, v, g, beta)
    zf = z.reshape(B, T, GDN_V_HEADS, GDN_HEAD_DIM).astype(jnp.float32)
    o = o * lax.rsqrt(jnp.mean(o * o, axis=-1, keepdims=True) + RMS_EPS) * norm_w.astype(jnp.float32) * jax.nn.silu(zf)
    return o.reshape(B, T, GDN_VALUE_DIM).astype(h.dtype) @ w_o


def moe_ffn(h, router_w, router_b, w_gu, b_gu, w_down, b_down):
    Bb, T, D = h.shape
    x2 = h.reshape(-1, D)
    n_tok = x2.shape[0]
    logits = (x2 @ router_w).astype(jnp.float32) + router_b.astype(jnp.float32)
    top_logit, top_e = lax.top_k(logits, TOP_K)
    gate = jax.nn.softmax(top_logit, axis=-1)
    n_slot = n_tok * TOP_K
    flat_e = top_e.reshape(-1)
    flat_tok = jnp.repeat(jnp.arange(n_tok, dtype=jnp.int32), TOP_K)
    flat_gate = gate.reshape(-1)
    order = jnp.argsort(flat_e)
    e_sorted = flat_e[order]
    counts = jnp.bincount(flat_e, length=N_EXPERTS)
    padded = (counts + MOE_BLOCK - 1) // MOE_BLOCK * MOE_BLOCK
    pad_end = jnp.cumsum(padded)
    pad_start = pad_end - padded
    start = jnp.cumsum(counts) - counts
    dest = pad_start[e_sorted] + jnp.arange(n_slot) - start[e_sorted]
    n_blocks = -(-n_slot // MOE_BLOCK) + N_EXPERTS
    n_rows = n_blocks * MOE_BLOCK
    row_tok = jnp.full((n_rows,), n_tok, dtype=jnp.int32).at[dest].set(flat_tok[order])
    row_gate = jnp.zeros((n_rows,), jnp.float32).at[dest].set(flat_gate[order])
    block_e = jnp.minimum(jnp.searchsorted(pad_end, jnp.arange(n_blocks) * MOE_BLOCK, side='right'), N_EXPERTS - 1)
    x_pad = jnp.concatenate([x2, jnp.zeros((1, D), x2.dtype)], axis=0)
    xb = x_pad[row_tok].reshape(n_blocks, MOE_BLOCK, D)

    def expert_block(args):
        xblk, e = args
        gu = xblk @ w_gu[e] + b_gu[e]
        glu = jnp.minimum(gu[:, 0::2], SWIGLU_LIMIT)
        lin = jnp.clip(gu[:, 1::2], -SWIGLU_LIMIT, SWIGLU_LIMIT)
        act = glu * jax.nn.sigmoid(SWIGLU_ALPHA * glu) * (lin + 1.0)
        return act @ w_down[e] + b_down[e]

    yb = lax.map(expert_block, (xb, block_e))
    y = jnp.zeros((n_tok + 1, D), jnp.float32).at[row_tok].add(
        yb.reshape(n_rows, D).astype(jnp.float32) * row_gate[:, None])
    return y[:n_tok].astype(h.dtype).reshape(Bb, T, D)


def setup_inputs(seed: int = 0) -> dict:
    key = jax.random.key(seed)
    ks = jax.random.split(key, 22)
    f32 = jnp.float32
    din = D_MODEL ** -0.5

    def nrm(k, shape, scale):
        return jax.random.normal(k, shape, f32) * scale

    x = nrm(ks[0], (BATCH, SEQ, D_MODEL), 1.0)
    sb_w_qkv = jnp.concatenate([
        nrm(ks[1], (N_SB_LAYERS, D_MODEL, 2 * SB_WIDTH), din),
        nrm(ks[2], (N_SB_LAYERS, D_MODEL, SB_WIDTH), din * DEEPNORM_BETA)], axis=-1)
    sb_w_o = nrm(ks[3], (N_SB_LAYERS, SB_WIDTH, D_MODEL), SB_WIDTH ** -0.5 * DEEPNORM_BETA)
    gdn_w_in = jnp.concatenate([
        nrm(ks[4], (N_GDN_LAYERS, D_MODEL, 2 * GDN_KEY_DIM), din),
        nrm(ks[5], (N_GDN_LAYERS, D_MODEL, GDN_VALUE_DIM), din * DEEPNORM_BETA),
        nrm(ks[6], (N_GDN_LAYERS, D_MODEL, GDN_VALUE_DIM + 2 * GDN_V_HEADS), din)], axis=-1)
    gdn_conv_w = nrm(ks[7], (N_GDN_LAYERS, GDN_CONV, GDN_CONV_CH), GDN_CONV ** -0.5)
    gdn_a_log = jnp.log(jax.random.uniform(ks[8], (N_GDN_LAYERS, GDN_V_HEADS), f32, 1.0, 16.0))
    dt = jnp.exp(jax.random.uniform(ks[9], (N_GDN_LAYERS, GDN_V_HEADS), f32, math.log(1e-3), math.log(1e-1)))
    gdn_dt_bias = dt + jnp.log(-jnp.expm1(-dt))
    gdn_norm_w = 1.0 + nrm(ks[10], (N_GDN_LAYERS, GDN_HEAD_DIM), 0.02)
    gdn_w_o = nrm(ks[11], (N_GDN_LAYERS, GDN_VALUE_DIM, D_MODEL), GDN_VALUE_DIM ** -0.5 * DEEPNORM_BETA)
    ln_mix_g = 1.0 + nrm(ks[12], (DEPTH, D_MODEL), 0.02)
    ln_mix_b = nrm(ks[13], (DEPTH, D_MODEL), 0.01)
    ln_ffn_g = 1.0 + nrm(ks[14], (DEPTH, D_MODEL), 0.02)
    ln_ffn_b = nrm(ks[15], (DEPTH, D_MODEL), 0.01)
    moe_router_w = nrm(ks[16], (DEPTH, D_MODEL, N_EXPERTS), din)
    moe_router_b = nrm(ks[17], (DEPTH, N_EXPERTS), 0.01)
    moe_w_gu = nrm(ks[18], (DEPTH, N_EXPERTS, D_MODEL, 2 * EXPERT_FF), din)
    moe_b_gu = nrm(ks[19], (DEPTH, N_EXPERTS, 2 * EXPERT_FF), 0.01)
    moe_w_down = nrm(ks[20], (DEPTH, N_EXPERTS, EXPERT_FF, D_MODEL), EXPERT_FF ** -0.5 * DEEPNORM_BETA)
    moe_b_down = nrm(ks[21], (DEPTH, N_EXPERTS, D_MODEL), 0.01)
    return {'x': x, 'sb_w_qkv': sb_w_qkv, 'sb_w_o': sb_w_o,
            'gdn_w_in': gdn_w_in, 'gdn_conv_w': gdn_conv_w, 'gdn_a_log': gdn_a_log,
            'gdn_dt_bias': gdn_dt_bias, 'gdn_norm_w': gdn_norm_w, 'gdn_w_o': gdn_w_o,
            'ln_mix_g': ln_mix_g, 'ln_mix_b': ln_mix_b, 'ln_ffn_g': ln_ffn_g, 'ln_ffn_b': ln_ffn_b,
            'moe_router_w': moe_router_w, 'moe_router_b': moe_router_b,
            'moe_w_gu': moe_w_gu, 'moe_b_gu': moe_b_gu, 'moe_w_down': moe_w_down, 'moe_b_down': moe_b_down}


def reference(x, sb_w_qkv, sb_w_o, gdn_w_in, gdn_conv_w, gdn_a_log, gdn_dt_bias, gdn_norm_w, gdn_w_o,
              ln_mix_g, ln_mix_b, ln_ffn_g, ln_ffn_b, moe_router_w, moe_router_b,
              moe_w_gu, moe_b_gu, moe_w_down, moe_b_down):
    for i in range(DEPTH):
        j = i // N_MIXERS
        if i % N_MIXERS == 0:
            mix = stick_breaking_attention(x, sb_w_qkv[j], sb_w_o[j])
        else:
            mix = gated_deltanet(x, gdn_w_in[j], gdn_conv_w[j], gdn_a_log[j], gdn_dt_bias[j],
                                 gdn_norm_w[j], gdn_w_o[j])
        x = layer_norm(DEEPNORM_ALPHA * x + mix, ln_mix_g[i], ln_mix_b[i])
        ffn = moe_ffn(x, moe_router_w[i], moe_router_b[i], moe_w_gu[i], moe_b_gu[i], moe_w_down[i], moe_b_down[i])
        x = layer_norm(DEEPNORM_ALPHA * x + ffn, ln_ffn_g[i], ln_ffn_b[i])
    return x
```

```python
from contextlib import ExitStack
import numpy as np
import ml_dtypes
import concourse.bass as bass
import concourse.mybir as mybir
from concourse.bass_utils import run_bass_kernel_spmd

F32 = mybir.dt.float32
BF16 = mybir.dt.bfloat16
I32 = mybir.dt.int32
AF = mybir.ActivationFunctionType
ALU = mybir.AluOpType
AX = mybir.AxisListType

D_MODEL = 2048
ALPHA = float(4 ** 0.25)
LN_EPS = 1e-5
NEXP = 32
CAP = 192

ENGS = ("pe", "act", "dve", "pool", "sp")


class Prog:
    def __init__(self, nc):
        self.nc = nc
        self.streams = {e: [] for e in ENGS}
        self.cnt = {e: 0 for e in ENGS}
        self.last_w = {}
        self.readers = {}
        self.waited = {e: {} for e in ENGS}
        self.dma_cnt = {}
        self.semnames = set(ENGS)

    def _collect(self, eng, reads, writes):
        deps = {}

        def add(tok):
            s, v = tok
            if deps.get(s, 0) < v:
                deps[s] = v

        for k in reads:
            t = self.last_w.get(k)
            if t:
                add(t)
        for k in writes:
            t = self.last_w.get(k)
            if t:
                add(t)
            for s, v in self.readers.get(k, {}).items():
                add((s, v))
        waits = []
        for s, v in deps.items():
            if eng == "pe" and s == "pe":
                continue
            if self.waited[eng].get(s, 0) < v:
                self.waited[eng][s] = v
                waits.append((s, v))
        return waits

    def _commit(self, tok, reads, writes):
        for k in writes:
            self.last_w[k] = tok
            self.readers[k] = {}
        for k in reads:
            if k in writes:
                continue
            r = self.readers.setdefault(k, {})
            if r.get(tok[0], 0) < tok[1]:
                r[tok[0]] = tok[1]

    mute = False
    _cap = None

    def begin_capture(self):
        self._cap = []

    def end_capture(self):
        c, self._cap = self._cap, None
        return c

    def merge(self, streams):
        its = [list(x) for x in streams if x]
        pos = [0] * len(its)
        live = True
        while live:
            live = False
            for i, x in enumerate(its):
                if pos[i] < len(x):
                    kind, eng, fn, reads, writes, sem = x[pos[i]]
                    pos[i] += 1
                    live = True
                    if kind == "op":
                        self.op(eng, fn, reads, writes, sig=(sem is not False))
                    else:
                        self.dma(eng, fn, reads, writes, sem)

    def op(self, eng, fn, reads=(), writes=(), sig=True):
        if self.mute:
            return
        if self._cap is not None:
            self._cap.append(("op", eng, fn, tuple(reads), tuple(writes), sig))
            return
        waits = self._collect(eng, reads, writes)
        if sig:
            self.cnt[eng] += 1
            tok = (eng, self.cnt[eng])
            self.streams[eng].append((waits, fn, tok[0], 1))
        else:
            tok = (eng, self.cnt[eng] + 1)
            self.streams[eng].append((waits, fn, None, 0))
        self._commit(tok, reads, writes)

    def dma(self, eng, fn, reads=(), writes=(), sem=None):
        if self.mute:
            return
        if sem is None:
            sem = writes[0] if writes else reads[0]
        if self._cap is not None:
            self._cap.append(("dma", eng, fn, tuple(reads), tuple(writes), sem))
            return
        sname = "d_" + sem
        self.semnames.add(sname)
        waits = self._collect(eng, reads, writes)
        prev = self.dma_cnt.get(sname, 0)
        if prev and self.waited[eng].get(sname, 0) < prev:
            self.waited[eng][sname] = prev
            waits.append((sname, prev))
        self.dma_cnt[sname] = prev + 16
        tok = (sname, prev + 16)
        self.streams[eng].append((waits, fn, sname, 16))
        self._commit(tok, reads, writes)

    def final_wait(self, eng, keys):
        waits = self._collect(eng, keys, ())
        self.streams[eng].append((waits, None, None, 0))

    def emit(self, stack):
        nc = self.nc
        sems = {}
        for n in sorted(self.semnames):
            sems[n] = stack.enter_context(nc.semaphore("s_" + n))

        def run(ename, e):
            for waits, fn, s, inc in self.streams[ename]:
                for (ws, wv) in waits:
                    e.wait_ge(sems[ws], wv)
                if fn is not None:
                    ins = fn(e)
                    if s is not None:
                        ins.then_inc(sems[s], inc)

        with nc.Block() as block:
            @block.tensor
            def _(e):
                run("pe", e)

            @block.scalar
            def _(e):
                run("act", e)

            @block.vector
            def _(e):
                run("dve", e)

            @block.gpsimd
            def _(e):
                run("pool", e)

            @block.sync
            def _(e):
                run("sp", e)


def _layernorm(T, nc, x, key, G, B, st, mv, rs, eps, tag):
    for c in range(4):
        T.op("dve", lambda e, c=c: e.bn_stats(out=st[:, c * 6:(c + 1) * 6], in_=x[:, c * 512:(c + 1) * 512]),
             reads=[key], writes=[tag + "st%d" % c])
    T.op("dve", lambda e: e.bn_aggr(out=mv[:, 0:2], in_=st[:, 0:24]),
         reads=[tag + "st%d" % c for c in range(4)], writes=[tag + "mv"])
    T.op("act", lambda e: e.activation(out=rs[:, 0:1], in_=mv[:, 1:2], func=AF.Sqrt, bias=eps[:, 0:1], scale=1.0),
         reads=[tag + "mv", "eps"], writes=[tag + "rs"])
    T.op("dve", lambda e: e.reciprocal(out=rs[:, 0:1], in_=rs[:, 0:1]), reads=[tag + "rs"], writes=[tag + "rs"])
    T.op("dve", lambda e: e.tensor_scalar(out=x, in0=x, scalar1=mv[:, 0:1], scalar2=rs[:, 0:1],
                                          op0=ALU.subtract, op1=ALU.mult),
         reads=[key, tag + "mv", tag + "rs"], writes=[key])
    T.op("dve", lambda e: e.tensor_tensor(out=x, in0=x, in1=G, op=ALU.mult), reads=[key, "LNGB"], writes=[key])
    T.op("dve", lambda e: e.tensor_tensor(out=x, in0=x, in1=B, op=ALU.add), reads=[key, "LNGB"], writes=[key])


NTILES = 8


def build_moe_r():
    nc = bass.Bass("TRN2", target_bir_lowering=False)
    ntiles = NTILES
    NT = ntiles * 128
    D = D_MODEL
    dt = nc.dram_tensor
    xin = dt("xin", [NT, D], F32, kind="ExternalInput").ap()
    p0 = dt("p0", [NT, D], F32, kind="ExternalInput").ap()
    p1 = dt("p1", [NT, D], F32, kind="ExternalInput").ap()
    lnp = dt("lnp", [2, D], F32, kind="ExternalInput").ap()
    rw = dt("rw", [128, 16 * 32], F32, kind="ExternalInput").ap()
    rb = dt("rb", [1, 32], F32, kind="ExternalInput").ap()
    cident = dt("cident", [128, 128], F32, kind="ExternalInput").ap()
    cstri = dt("cstri", [128, 128], BF16, kind="ExternalInput").ap()
    ciota = dt("ciota", [128, CAP], F32, kind="ExternalInput").ap()
    cioe = dt("cioe", [128, 32], F32, kind="ExternalInput").ap()
    x1o = dt("x1o", [NT, D], F32, kind="ExternalOutput").ap()
    dsp = dt("dsp", [NEXP, 128, 16 * CAP], BF16, kind="ExternalOutput").ap()
    aidx_o = dt("aidx", [128, ntiles * 4], I32, kind="ExternalOutput").ap()
    gate_o = dt("gate", [128, ntiles * 4], F32, kind="ExternalOutput").ap()

    with ExitStack() as st:
        def sb(name, shape, dtype):
            return st.enter_context(nc.sbuf_tensor(name, shape, dtype))

        X1B = sb("X1B", [128, ntiles, D], BF16)
        POS = sb("POS", [128, ntiles, 32], F32)
        GATE = sb("GATE", [128, ntiles, 4], F32)
        AIDX = sb("AIDX", [128, ntiles, 4], I32)
        xa = sb("xa", [128, D], F32)
        pa = sb("pa", [128, D], F32)
        pb = sb("pb", [128, D], F32)
        pc = sb("pc", [128, D], F32)
        LNGB = sb("LNGB", [128, 2, D], F32)
        SEL = [sb("SEL%d" % i, [128, ntiles, CAP], BF16) for i in range(2)]
        XG = [sb("XG%d" % i, [128, 16, CAP], BF16) for i in range(2)]
        RW = sb("RW", [128, 16 * 32], F32)
        RB = sb("RB", [128, 32], F32)
        IDENT = sb("IDENT", [128, 128], F32)
        STRI = sb("STRI", [128, 128], BF16)
        ONES = sb("ONES", [128, 128], BF16)
        IOTA = sb("IOTA", [128, CAP], F32)
        IOE = sb("IOE", [128, 32], F32)
        EPS = sb("EPS", [128, 1], F32)
        stt = sb("stt", [128, 24], F32)
        mv = sb("mv", [128, 2], F32)
        rs = sb("rs", [128, 1], F32)
        lg = sb("lg", [128, 32], F32)
        top8 = sb("top8", [128, 8], F32)
        mkb = sb("mkb", [128, 32], BF16)
        cum = sb("cum", [128, 32], BF16)
        t32 = sb("t32", [128, 32], F32)
        addr = sb("addr", [128, 32], F32)
        oh = sb("oh", [128, 32], F32)
        af = sb("af", [128, 4], F32)
        nmax = sb("nmax", [128, 1], F32)
        g4 = sb("g4", [128, 4], F32)
        den = sb("den", [128, 1], F32)
        PS = [st.enter_context(nc.psum_tensor("ps%d" % i, [128, 512], F32)) for i in range(8)]

        T = Prog(nc)
        G_t, B_t = LNGB[:, 0, :], LNGB[:, 1, :]
        T.dma("sp", lambda e: e.dma_start(out=RW[:], in_=rw), writes=["RW"])
        T.dma("sp", lambda e: e.dma_start(out=RB[:], in_=rb.partition_broadcast(128)), writes=["RB"])
        T.dma("sp", lambda e: e.dma_start(out=IDENT[:], in_=cident), writes=["IDENT"])
        T.dma("sp", lambda e: e.dma_start(out=STRI[:], in_=cstri), writes=["STRI"])
        T.dma("sp", lambda e: e.dma_start(out=IOTA[:], in_=ciota), writes=["IOTA"])
        T.dma("sp", lambda e: e.dma_start(out=IOE[:], in_=cioe), writes=["IOE"])
        T.op("dve", lambda e: e.memset(ONES[:], 1.0), writes=["ONES"])
        T.op("dve", lambda e: e.memset(EPS[:], LN_EPS), writes=["eps"])
        T.op("dve", lambda e: e.memset(cum[:], 0.0), writes=["cum"])
        T.dma("sp", lambda e: e.dma_start(out=G_t, in_=lnp[0:1, :].partition_broadcast(128)), writes=["LNGB"], sem="lnG")
        T.dma("sp", lambda e: e.dma_start(out=B_t, in_=lnp[1:2, :].partition_broadcast(128)), writes=["LNGB"], sem="lnB")

        x1T = pc[:].rearrange("p (c t) -> p c t", c=16)
        for i in range(ntiles):
            rows = slice(i * 128, (i + 1) * 128)
            T.dma("sp", lambda e, rows=rows: e.dma_start(out=xa[:], in_=xin[rows, :]), writes=["xa"])
            T.dma("sp", lambda e, rows=rows: e.dma_start(out=pa[:], in_=p0[rows, :]), writes=["pa"])
            T.dma("sp", lambda e, rows=rows: e.dma_start(out=pb[:], in_=p1[rows, :]), writes=["pb"])
            T.op("dve", lambda e: e.scalar_tensor_tensor(out=xa[:], in0=xa[:], scalar=ALPHA, in1=pa[:],
                                                         op0=ALU.mult, op1=ALU.add),
                 reads=["xa", "pa"], writes=["xa"])
            T.op("dve", lambda e: e.tensor_tensor(out=xa[:], in0=xa[:], in1=pb[:], op=ALU.add),
                 reads=["xa", "pb"], writes=["xa"])
            _layernorm(T, nc, xa[:], "xa", G_t, B_t, stt, mv, rs, EPS, "l1")
            T.dma("sp", lambda e, rows=rows: e.dma_start(out=x1o[rows, :], in_=xa[:]), reads=["xa"], writes=["out"], sem="out")
            T.op("act", lambda e, i=i: e.copy(out=X1B[:, i, :], in_=xa[:]), reads=["xa"], writes=["X1B"])
            for q in range(4):
                bank = PS[6 + (q % 2)]
                bk = "ps%d" % (6 + (q % 2))
                for j in range(4):
                    fc = q * 4 + j
                    T.op("pe", lambda e, bank=bank, j=j, fc=fc: e.transpose(
                        out=bank[:, j * 128:(j + 1) * 128], in_=xa[:, fc * 128:(fc + 1) * 128], identity=IDENT[:]),
                        reads=["xa", "IDENT"], writes=[bk])
                T.op("act", lambda e, bank=bank, q=q: e.copy(
                    out=pc[:, q * 512:(q + 1) * 512], in_=bank[:, :]), reads=[bk], writes=["pc"])
            for fc in range(16):
                T.op("pe", lambda e, fc=fc: e.matmul(PS[5][:, 0:32], lhsT=x1T[:, fc, :], rhs=RW[:, fc * 32:(fc + 1) * 32],
                                                     start=(fc == 0), stop=(fc == 15)),
                     reads=["pc", "RW"], writes=["ps5"])
            T.op("dve", lambda e: e.tensor_tensor(out=lg[:], in0=PS[5][:, 0:32], in1=RB[:], op=ALU.add),
                 reads=["ps5", "RB"], writes=["lg"])
            T.op("dve", lambda e: e.max(out=top8[:], in_=lg[:]), reads=["lg"], writes=["top8"])
            T.op("dve", lambda e: e.tensor_scalar(out=mkb[:], in0=lg[:], scalar1=top8[:, 3:4], scalar2=None, op0=ALU.is_ge),
                 reads=["lg", "top8"], writes=["mkb"])
            T.op("pe", lambda e, i=i: e.matmul(PS[4][:, 0:32], lhsT=STRI[:], rhs=mkb[:], start=True, stop=(i == 0)),
                 reads=["STRI", "mkb"], writes=["ps4"])
            if i > 0:
                T.op("pe", lambda e: e.matmul(PS[4][:, 0:32], lhsT=ONES[:], rhs=cum[:], start=False, stop=True),
                     reads=["ONES", "cum"], writes=["ps4"])
            T.op("dve", lambda e: e.tensor_tensor(out=cum[:], in0=cum[:], in1=mkb[:], op=ALU.add),
                 reads=["cum", "mkb"], writes=["cum"])
            T.op("dve", lambda e: e.scalar_tensor_tensor(out=t32[:], in0=PS[4][:, 0:32], scalar=1.0, in1=mkb[:],
                                                         op0=ALU.add, op1=ALU.mult),
                 reads=["ps4", "mkb"], writes=["t32"])
            T.op("dve", lambda e, i=i: e.tensor_scalar(out=POS[:, i, :], in0=t32[:], scalar1=-1.0, scalar2=None, op0=ALU.add),
                 reads=["t32"], writes=["POS"])
            T.op("dve", lambda e: e.tensor_scalar(out=nmax[:], in0=top8[:, 0:1], scalar1=-1.0, scalar2=None, op0=ALU.mult),
                 reads=["top8"], writes=["nmax"])
            T.op("act", lambda e: e.activation(out=g4[:], in_=top8[:, 0:4], func=AF.Exp, bias=nmax[:, 0:1], scale=1.0),
                 reads=["top8", "nmax"], writes=["g4"])
            T.op("dve", lambda e: e.tensor_reduce(out=den[:], in_=g4[:], axis=AX.X, op=ALU.add), reads=["g4"], writes=["den"])
            T.op("dve", lambda e: e.reciprocal(out=den[:], in_=den[:]), reads=["den"], writes=["den"])
            T.op("dve", lambda e, i=i: e.tensor_scalar(out=GATE[:, i, :], in0=g4[:], scalar1=den[:, 0:1], scalar2=None, op0=ALU.mult),
                 reads=["g4", "den"], writes=["GATE"])
            T.op("dve", lambda e, i=i: e.tensor_tensor(out=addr[:], in0=IOE[:], in1=POS[:, i, :], op=ALU.add),
                 reads=["IOE", "POS"], writes=["addr"])
            for k in range(4):
                T.op("dve", lambda e, k=k: e.tensor_scalar(out=oh[:], in0=lg[:], scalar1=top8[:, k:k + 1], scalar2=None,
                                                           op0=ALU.is_equal), reads=["lg", "top8"], writes=["oh"])
                T.op("dve", lambda e: e.tensor_tensor(out=oh[:], in0=oh[:], in1=addr[:], op=ALU.mult),
                     reads=["oh", "addr"], writes=["oh"])
                T.op("dve", lambda e, k=k: e.tensor_reduce(out=af[:, k:k + 1], in_=oh[:], axis=AX.X, op=ALU.add),
                     reads=["oh"], writes=["af"])
            T.op("dve", lambda e, i=i: e.tensor_copy(out=AIDX[:, i, :], in_=af[:]), reads=["af"], writes=["AIDX"])
        T.dma("sp", lambda e: e.dma_start(out=aidx_o, in_=AIDX[:].rearrange("p a b -> p (a b)")), reads=["AIDX"], writes=["out"], sem="out")
        T.dma("sp", lambda e: e.dma_start(out=gate_o, in_=GATE[:].rearrange("p a b -> p (a b)")), reads=["GATE"], writes=["out"], sem="out")

        for ex in range(NEXP):
            b = ex % 2
            sel, xg = SEL[b], XG[b]
            ksel, kxg = "SEL%d" % b, "XG%d" % b
            for i in range(ntiles):
                T.op("dve", lambda e, i=i, sel=sel, ex=ex: e.tensor_scalar(
                    out=sel[:, i, :], in0=IOTA[:], scalar1=POS[:, i, ex:ex + 1], scalar2=None, op0=ALU.is_equal),
                    reads=["IOTA", "POS"], writes=[ksel])
            for q in range(8):
                bank = PS[q % 4]
                bk = "ps%d" % (q % 4)
                for j in range(2):
                    fc = q * 2 + j
                    for i in range(ntiles):
                        T.op("pe", lambda e, bank=bank, j=j, fc=fc, i=i, sel=sel: e.matmul(
                            bank[:, j * CAP:(j + 1) * CAP], lhsT=X1B[:, i, fc * 128:(fc + 1) * 128], rhs=sel[:, i, :],
                            start=(i == 0), stop=(i == ntiles - 1)),
                            reads=["X1B", ksel], writes=[bk], sig=(i == ntiles - 1))
                T.op("act", lambda e, bank=bank, q=q, xg=xg: e.copy(
                    out=xg[:, 2 * q:2 * q + 2, :], in_=bank[:, 0:2 * CAP].rearrange("p (a b) -> p a b", a=2)),
                    reads=[bk], writes=[kxg])
            T.dma("sp", lambda e, ex=ex, xg=xg: e.dma_start(out=dsp[ex], in_=xg[:].rearrange("p a b -> p (a b)")),
                  reads=[kxg], writes=["out"], sem="dsp%d" % b)
        T.final_wait("sp", ["out"])
        T.streams["sp"].append(([(n, v) for n, v in T.dma_cnt.items() if n in ("d_out", "d_dsp0", "d_dsp1")], None, None, 0))
        T.emit(st)
    return nc


NLOC = 4
NSLOT = 8 * CAP


def build_moe_x():
    nc = bass.Bass("TRN2", target_bir_lowering=False)
    D = D_MODEL
    dt = nc.dram_tensor
    xg_in = dt("xg", [NLOC, 8, 128, 16 * CAP], BF16, kind="ExternalInput").ap()
    wgl = dt("wgl", [NLOC, 16, 128, 2 * 16 * 128], F32, kind="ExternalInput").ap()
    wd = dt("wd", [NLOC, 4, 128, 16 * 512], F32, kind="ExternalInput").ap()
    bgl = dt("bgl", [128, NLOC * 32], F32, kind="ExternalInput").ap()
    bd = dt("bd", [NLOC, D], F32, kind="ExternalInput").ap()
    yout = dt("y", [NLOC, NSLOT, D], F32, kind="ExternalOutput").ap()
    NSC = NSLOT // 512
    NST = NSLOT // 128

    with ExitStack() as st:
        def sb(name, shape, dtype):
            return st.enter_context(nc.sbuf_tensor(name, shape, dtype))

        XG = sb("XG", [128, 16, NSLOT], BF16)
        ACTT = sb("ACTT", [128, 16, NSLOT], BF16)
        WGL = [sb("WGL%d" % i, [128, 2 * 16 * 128], BF16) for i in range(2)]
        WD = [sb("WD%d" % i, [128, 16 * 512], BF16) for i in range(2)]
        BD = sb("BD", [128, D], F32)
        BGL = sb("BGL", [128, NLOC * 32], F32)
        YST = [sb("YST%d" % i, [128, 512], F32) for i in range(4)]
        tg = [sb("tg%d" % i, [128, 512], F32) for i in range(2)]
        tl = [sb("tl%d" % i, [128, 512], F32) for i in range(2)]
        tsg = [sb("tsg%d" % i, [128, 512], F32) for i in range(2)]
        PS = [st.enter_context(nc.psum_tensor("ps%d" % i, [128, 512], F32)) for i in range(8)]

        T = Prog(nc)
        T.dma("sp", lambda e: e.dma_start(out=BGL[:], in_=bgl), writes=["BGL"])

        steps = []
        for ex in range(NLOC):
            for ffc in range(16):
                steps.append(("gl", ex, ffc))
            for ncn in range(4):
                steps.append(("d", ex, ncn))
        nload = [0]
        cntk = {"gl": 0, "d": 0}
        slot_of = {}

        def issue_loads(upto):
            while nload[0] < len(steps) and nload[0] <= upto:
                kind, ex, idx = steps[nload[0]]
                s = cntk[kind] % 2
                cntk[kind] += 1
                slot_of[nload[0]] = s
                if kind == "gl":
                    T.dma("pool", lambda e, s=s, ex=ex, idx=idx: e.dma_start(out=WGL[s][:], in_=wgl[ex, idx]),
                          writes=["WGL%d" % s])
                else:
                    if idx == 0:
                        T.dma("sp", lambda e, ex=ex: e.dma_start(out=BD[:], in_=bd[ex:ex + 1, :].partition_broadcast(128)),
                              writes=["BD"])
                    T.dma("pool", lambda e, s=s, ex=ex, idx=idx: e.dma_start(out=WD[s][:], in_=wd[ex, idx]),
                          writes=["WD%d" % s])
                nload[0] += 1

        si = 0
        yi = 0
        for ex in range(NLOC):
            for src in range(8):
                T.dma("sp", lambda e, ex=ex, src=src: e.dma_start(
                    out=XG[:, :, src * CAP:(src + 1) * CAP], in_=xg_in[ex, src].rearrange("p (a b) -> p a b", a=16)),
                    writes=["XG"], sem="XG%d" % src)
            for ffc in range(16):
                issue_loads(si + 1)
                s = slot_of[si]
                si += 1
                w = WGL[s]
                bgc = (ex * 2 + 0) * 16 + ffc
                blc = (ex * 2 + 1) * 16 + ffc
                for sc in range(NSC):
                    h = (ffc * NSC + sc) % 2
                    bg, bl = PS[2 * h], PS[2 * h + 1]
                    kg, kl = "ps%d" % (2 * h), "ps%d" % (2 * h + 1)
                    for gl, bank, bk in ((0, bg, kg), (1, bl, kl)):
                        for fc in range(16):
                            off = (gl * 16 + fc) * 128
                            T.op("pe", lambda e, bank=bank, fc=fc, off=off, w=w, sc=sc: e.matmul(
                                bank[:, :], lhsT=w[:, off:off + 128], rhs=XG[:, fc, sc * 512:(sc + 1) * 512],
                                start=(fc == 0), stop=(fc == 15)),
                                reads=["WGL%d" % s, "XG"], writes=[bk], sig=(fc == 15))
                    T.op("dve", lambda e, bg=bg, h=h, bgc=bgc: e.tensor_scalar(
                        out=tg[h][:], in0=bg[:, :], scalar1=BGL[:, bgc:bgc + 1], scalar2=7.0, op0=ALU.add, op1=ALU.min),
                        reads=[kg, "BGL"], writes=["tg%d" % h])
                    T.op("dve", lambda e, bl=bl, h=h, blc=blc: e.tensor_scalar(
                        out=tl[h][:], in0=bl[:, :], scalar1=BGL[:, blc:blc + 1], scalar2=7.0, op0=ALU.add, op1=ALU.min),
                        reads=[kl, "BGL"], writes=["tl%d" % h])
                    T.op("act", lambda e, h=h: e.activation(out=tsg[h][:], in_=tg[h][:], func=AF.Sigmoid, scale=1.702),
                         reads=["tg%d" % h], writes=["tsg%d" % h])
                    T.op("dve", lambda e, h=h: e.tensor_scalar(out=tl[h][:], in0=tl[h][:], scalar1=-7.0, scalar2=1.0,
                                                                op0=ALU.max, op1=ALU.add),
                         reads=["tl%d" % h], writes=["tl%d" % h])
                    T.op("dve", lambda e, h=h: e.tensor_tensor(out=tg[h][:], in0=tg[h][:], in1=tsg[h][:], op=ALU.mult),
                         reads=["tg%d" % h, "tsg%d" % h], writes=["tg%d" % h])
                    T.op("dve", lambda e, h=h, ffc=ffc, sc=sc: e.tensor_tensor(
                        out=ACTT[:, ffc, sc * 512:(sc + 1) * 512], in0=tg[h][:], in1=tl[h][:], op=ALU.mult),
                        reads=["tg%d" % h, "tl%d" % h], writes=["ACTT"])
            for ncn in range(4):
                issue_loads(si + 1)
                s = slot_of[si]
                si += 1
                w = WD[s]
                for stl in range(NST):
                    bank = PS[4 + (stl % 4)]
                    bk = "ps%d" % (4 + (stl % 4))
                    for jc in range(16):
                        T.op("pe", lambda e, bank=bank, jc=jc, w=w, stl=stl: e.matmul(
                            bank[:, :], lhsT=ACTT[:, jc, stl * 128:(stl + 1) * 128], rhs=w[:, jc * 512:(jc + 1) * 512],
                            start=(jc == 0), stop=(jc == 15)),
                            reads=["ACTT", "WD%d" % s], writes=[bk], sig=(jc == 15))
                    ys = YST[yi % 4]
                    ky = "YST%d" % (yi % 4)
                    yi += 1
                    T.op("dve", lambda e, bank=bank, ys=ys, ncn=ncn: e.tensor_tensor(
                        out=ys[:], in0=bank[:, :], in1=BD[:, ncn * 512:(ncn + 1) * 512], op=ALU.add),
                        reads=[bk, "BD"], writes=[ky])
                    T.dma("sp", lambda e, ex=ex, stl=stl, ncn=ncn, ys=ys: e.dma_start(
                        out=yout[ex, stl * 128:(stl + 1) * 128, ncn * 512:(ncn + 1) * 512], in_=ys[:]),
                        reads=[ky], writes=["out"], sem="o" + ky)
        T.streams["sp"].append(([(n, v) for n, v in T.dma_cnt.items() if n.startswith("d_oYST")], None, None, 0))
        T.emit(st)
    return nc


def build_moe_c():
    nc = bass.Bass("TRN2", target_bir_lowering=False)
    ntiles = NTILES
    NT = ntiles * 128
    D = D_MODEL
    dt = nc.dram_tensor
    x1 = dt("x1", [NT, D], F32, kind="ExternalInput").ap()
    ybuf = dt("ybuf", [NEXP * CAP, D], F32, kind="ExternalInput").ap()
    aidx_i = dt("aidx", [128, ntiles * 4], I32, kind="ExternalInput").ap()
    gate_i = dt("gate", [128, ntiles * 4], F32, kind="ExternalInput").ap()
    lnp = dt("lnp", [2, D], F32, kind="ExternalInput").ap()
    out = dt("out", [NT, D], F32, kind="ExternalOutput").ap()
    with ExitStack() as st:
        def sb(name, shape, dtype):
            return st.enter_context(nc.sbuf_tensor(name, shape, dtype))
        GATE = sb("GATE", [128, ntiles * 4], F32)
        AIDX = sb("AIDX", [128, ntiles * 4], I32)
        LNGB = sb("LNGB", [128, 2, D], F32)
        xa = [sb("xa%d" % i, [128, D], F32) for i in range(2)]
        gb = [sb("gb%d" % i, [128, D], F32) for i in range(4)]
        EPS = sb("EPS", [128, 1], F32)
        stt = sb("stt", [128, 24], F32)
        mv = sb("mv", [128, 2], F32)
        rs = sb("rs", [128, 1], F32)
        T = Prog(nc)
        G_t, B_t = LNGB[:, 0, :], LNGB[:, 1, :]
        T.op("dve", lambda e: e.memset(EPS[:], LN_EPS), writes=["eps"])
        T.dma("sp", lambda e: e.dma_start(out=G_t, in_=lnp[0:1, :].partition_broadcast(128)), writes=["LNGB"], sem="lnG")
        T.dma("sp", lambda e: e.dma_start(out=B_t, in_=lnp[1:2, :].partition_broadcast(128)), writes=["LNGB"], sem="lnB")
        T.dma("sp", lambda e: e.dma_start(out=GATE[:], in_=gate_i), writes=["GATE"])
        T.dma("sp", lambda e: e.dma_start(out=AIDX[:], in_=aidx_i), writes=["AIDX"])
        for i in range(ntiles):
            rows = slice(i * 128, (i + 1) * 128)
            x = xa[i % 2]
            kx = "xa%d" % (i % 2)
            T.dma("sp", lambda e, rows=rows, x=x: e.dma_start(out=x[:], in_=x1[rows, :]), writes=[kx])
            for k in range(4):
                T.dma("pool", lambda e, i=i, k=k: e.indirect_dma_start(
                    out=gb[k][:, :], out_offset=None, in_=ybuf[:, :],
                    in_offset=bass.IndirectOffsetOnAxis(ap=AIDX[:, i * 4 + k:i * 4 + k + 1], axis=0)),
                    reads=["AIDX"], writes=["gb%d" % k])
            T.op("dve", lambda e, x=x: e.tensor_scalar(out=x[:], in0=x[:], scalar1=ALPHA, scalar2=None, op0=ALU.mult),
                 reads=[kx], writes=[kx])
            for k in range(4):
                T.op("dve", lambda e, i=i, k=k, x=x: e.scalar_tensor_tensor(
                    out=x[:], in0=gb[k][:], scalar=GATE[:, i * 4 + k:i * 4 + k + 1], in1=x[:], op0=ALU.mult, op1=ALU.add),
                    reads=["gb%d" % k, "GATE", kx], writes=[kx])
            _layernorm(T, nc, x[:], kx, G_t, B_t, stt, mv, rs, EPS, "l2")
            T.dma("sp", lambda e, rows=rows, x=x: e.dma_start(out=out[rows, :], in_=x[:]), reads=[kx], writes=["out"], sem="out%d" % (i % 2))
        T.streams["sp"].append(([(n, v) for n, v in T.dma_cnt.items() if n.startswith("d_out")], None, None, 0))
        T.emit(st)
    return nc


def moe_consts():
    bf = ml_dtypes.bfloat16
    c = {}
    c["cident"] = np.eye(128, dtype=np.float32)
    c["cstri"] = np.triu(np.ones((128, 128), np.float32), 1).astype(bf)
    c["ciota"] = np.tile(np.arange(CAP, dtype=np.float32)[None, :], (128, 1))
    c["cioe"] = np.tile((np.arange(32, dtype=np.float32) * CAP)[None, :], (128, 1))
    return c


def moe_weights(router_w, router_b, w_gu, b_gu, w_down, b_down):
    E = w_gu.shape[0]
    r = {}
    r["rw"] = np.ascontiguousarray(router_w.reshape(16, 128, 32).transpose(1, 0, 2)).reshape(128, 16 * 32)
    r["rb"] = np.ascontiguousarray(router_b.reshape(1, 32))
    per = []
    for c in range(E // NLOC):
        es = slice(c * NLOC, (c + 1) * NLOC)
        d = {}
        w = w_gu[es].reshape(NLOC, 16, 128, 16, 128, 2)
        d["wgl"] = np.ascontiguousarray(w.transpose(0, 3, 2, 5, 1, 4)).reshape(NLOC, 16, 128, 2 * 16 * 128)
        w = w_down[es].reshape(NLOC, 16, 128, 4, 512)
        d["wd"] = np.ascontiguousarray(w.transpose(0, 3, 2, 1, 4)).reshape(NLOC, 4, 128, 16 * 512)
        b = b_gu[es].reshape(NLOC, 16, 128, 2)
        d["bgl"] = np.ascontiguousarray(b.transpose(2, 0, 3, 1)).reshape(128, NLOC * 32)
        d["bd"] = np.ascontiguousarray(b_down[es])
        per.append(d)
    return r, per


def run_moe_layer(progs, xin_l, p0_l, p1_l, ln1, ln2, rdict, wper, consts):
    nR, nX, nC = progs
    ims = []
    for c in range(8):
        im = dict(xin=xin_l[c], p0=p0_l[c], p1=p1_l[c], lnp=ln1)
        im.update(rdict)
        im.update(consts)
        ims.append(im)
    rr = run_bass_kernel_spmd(nR, ims, core_ids=list(range(8))).results
    ims = []
    for j in range(8):
        xg = np.stack([np.stack([rr[src]["dsp"][NLOC * j + el] for src in range(8)]) for el in range(NLOC)])
        im = dict(xg=xg)
        im.update(wper[j])
        ims.append(im)
    xr = run_bass_kernel_spmd(nX, ims, core_ids=list(range(8))).results
    ims = []
    for c in range(8):
        yb = np.concatenate([xr[e // NLOC]["y"][e % NLOC, c * CAP:(c + 1) * CAP] for e in range(NEXP)], axis=0)
        ims.append(dict(x1=rr[c]["x1o"], ybuf=yb, aidx=rr[c]["aidx"], gate=rr[c]["gate"], lnp=ln2))
    cr = run_bass_kernel_spmd(nC, ims, core_ids=list(range(8))).results
    return [cr[c]["out"] for c in range(8)]


HL = 8
SEQ = 2048


def build_att(att=True, nh=HL, nqc=4, parts='qkv'):
    nc = bass.Bass("TRN2", target_bir_lowering=False)
    D = D_MODEL
    dt = nc.dram_tensor
    xT = dt("xT", [16, 128, SEQ], F32, kind="ExternalInput").ap()
    wqkv = dt("wqkv", [HL, 128, 3 * 16 * 128], F32, kind="ExternalInput").ap()
    wo = dt("wo", [128, HL * D], F32, kind="ExternalInput").ap()
    cmask = dt("cmask", [128, 896], BF16, kind="ExternalInput").ap()
    ctri = dt("ctri", [128, 128], BF16, kind="ExternalInput").ap()
    pout = dt("pout", [SEQ, D], F32, kind="ExternalOutput").ap()
    SC = float(128 ** -0.5)
    with ExitStack() as st:
        def sb(name, shape, dtype):
            return st.enter_context(nc.sbuf_tensor(name, shape, dtype))
        XT = sb("XT", [128, 16, SEQ], BF16)
        W = [sb("W%d" % i, [128, 3 * 16 * 128], BF16) for i in range(2)]
        WO = sb("WO", [128, HL * 512], BF16)
        QT = [sb("QT%d" % i, [128, SEQ], BF16) for i in range(4)]
        KT = [sb("KT%d" % i, [128, SEQ], BF16) for i in range(4)]
        V = [sb("V%d" % i, [128, 16, 128], BF16) for i in range(4)]
        OT = sb("OT", [128, HL, SEQ], BF16)
        U = [sb("U%d" % i, [128, 512], F32) for i in range(2)]
        SPM = [sb("SPM%d" % i, [128, 512], BF16) for i in range(2)]
        SPF = [sb("SPF%d" % i, [128, 512], F32) for i in range(2)]
        WW = [sb("WW%d" % i, [128, 512], BF16) for i in range(2)]
        R = [sb("R%d" % i, [128, 512], BF16) for i in range(2)]
        MB = sb("MB", [128, 896], BF16)
        TRI = sb("TRI", [128, 128], BF16)
        ONES = sb("ONES", [128, 128], BF16)
        ONE1 = sb("ONE1", [128, 1], F32)
        OST = [sb("OST%d" % i, [128, 512], F32) for i in range(2)]
        PS = [st.enter_context(nc.psum_tensor("ps%d" % i, [128, 512], F32)) for i in range(8)]
        T = Prog(nc)
        T.dma("sp", lambda e: e.dma_start(out=MB[:], in_=cmask), writes=["MB"])
        T.dma("sp", lambda e: e.dma_start(out=TRI[:], in_=ctri), writes=["TRI"])
        T.op("dve", lambda e: e.memset(ONES[:], -1.0), writes=["ONES"])
        T.op("dve", lambda e: e.memset(ONE1[:], 1.0), writes=["ONE1"])
        for fc in range(16):
            T.dma("pool", lambda e, fc=fc: e.dma_start(out=XT[:, fc, :], in_=xT[fc]), writes=["XT%d" % fc])
        xtk = ["XT%d" % fc for fc in range(16)]
        if not att or nh < HL or nqc < 4:
            T.op("dve", lambda e: e.memset(OT[:], 0.0), writes=["OT"])

        def emit_proj(hl):
            p = hl % 2
            bs = 2 * ((hl // 2) % 2) + p
            w, kw = W[p], "W%d" % p
            zb, cb, ob, pb = PS[4 * p], PS[4 * p + 1], PS[4 * p + 2], PS[4 * p + 3]
            zk, ck, okey, pk = "ps%d" % (4 * p), "ps%d" % (4 * p + 1), "ps%d" % (4 * p + 2), "ps%d" % (4 * p + 3)
            kq, knq, kk, kv = "QT%d" % bs, "NQT%d" % bs, "KT%d" % bs, "V%d" % bs
            ku, kspf, kspm, kww, kr = "U%d" % p, "SPF%d" % p, "SPM%d" % p, "WW%d" % p, "R%d" % p
            T.dma("pool", lambda e: e.dma_start(out=w[:], in_=wqkv[hl]), writes=[kw])
            for which in (0, 1):
                if "qk"[which] not in parts:
                    continue
                for tcn in range(4):
                    cs_ = slice(tcn * 512, (tcn + 1) * 512)
                    for fc in range(16):
                        off = (which * 16 + fc) * 128
                        T.op("pe", lambda e, fc=fc, off=off, cs_=cs_: e.matmul(
                            pb[:, :], lhsT=w[:, off:off + 128], rhs=XT[:, fc, cs_], start=(fc == 0), stop=(fc == 15)),
                            reads=[kw, "XT%d" % fc], writes=[pk], sig=(fc == 15))
                    if which == 0:
                        T.op("dve", lambda e, cs_=cs_: e.tensor_scalar(out=QT[bs][:, cs_], in0=pb[:, :], scalar1=SC, scalar2=None, op0=ALU.mult),
                             reads=[pk], writes=[kq])
                    else:
                        T.op("dve", lambda e, cs_=cs_: e.tensor_copy(out=KT[bs][:, cs_], in_=pb[:, :]), reads=[pk], writes=[kk])
            for q in range(4 if "v" in parts else 0):
                for j in range(4):
                    tt = q * 4 + j
                    for fc in range(16):
                        off = (2 * 16 + fc) * 128
                        T.op("pe", lambda e, j=j, tt=tt, fc=fc, off=off: e.matmul(
                            pb[:, j * 128:(j + 1) * 128], lhsT=XT[:, fc, tt * 128:(tt + 1) * 128], rhs=w[:, off:off + 128],
                            start=(fc == 0), stop=(fc == 15)), reads=[kw, "XT%d" % fc], writes=[pk], sig=(fc == 15))
                T.op("dve", lambda e, q=q: e.tensor_copy(out=V[bs][:, 4 * q:4 * q + 4, :], in_=pb[:, :].rearrange("p (a b) -> p a b", a=4)),
                     reads=[pk], writes=[kv])
        def emit_att(hl):
            p = hl % 2
            bs = 2 * ((hl // 2) % 2) + p
            w, kw = W[p], "W%d" % p
            zb, cb, ob, pb = PS[4 * p], PS[4 * p + 1], PS[4 * p + 2], PS[4 * p + 3]
            zk, ck, okey, pk = "ps%d" % (4 * p), "ps%d" % (4 * p + 1), "ps%d" % (4 * p + 2), "ps%d" % (4 * p + 3)
            kq, knq, kk, kv = "QT%d" % bs, "NQT%d" % bs, "KT%d" % bs, "V%d" % bs
            ku, kspf, kspm, kww, kr = "U%d" % p, "SPF%d" % p, "SPM%d" % p, "WW%d" % p, "R%d" % p
            for qc in range(nqc if att else 0):
                ktop = 4 * qc + 3
                qs = slice(qc * 512, (qc + 1) * 512)
                for kb in range(ktop, -1, -1):
                    ks = slice(kb * 128, (kb + 1) * 128)
                    r = kb - 4 * qc
                    first = (kb == ktop)
                    msl = slice(384 - 128 * r, 384 - 128 * r + 512) if r >= 0 else None
                    T.op("pe", lambda e, ks=ks, qs=qs: e.matmul(zb[:, :], lhsT=KT[bs][:, ks], rhs=QT[bs][:, qs], start=True, stop=True),
                         reads=[kk, kq], writes=[zk])
                    T.op("act", lambda e: e.activation(out=U[p][:], in_=zb[:, :], func=AF.Exp), reads=[zk], writes=[ku])
                    if r >= 0:
                        T.op("act", lambda e: e.activation(out=SPF[p][:], in_=U[p][:], func=AF.Ln, bias=ONE1[:, 0:1], scale=1.0),
                             reads=[ku, "ONE1"], writes=[kspf])
                        T.op("dve", lambda e, msl=msl: e.tensor_tensor(out=SPM[p][:], in0=SPF[p][:], in1=MB[:, msl], op=ALU.mult),
                             reads=[kspf, "MB"], writes=[kspm])
                    else:
                        T.op("act", lambda e: e.activation(out=SPM[p][:], in_=U[p][:], func=AF.Ln, bias=ONE1[:, 0:1], scale=1.0),
                             reads=[ku, "ONE1"], writes=[kspm])
                    T.op("pe", lambda e: e.matmul(cb[:, :], lhsT=TRI[:], rhs=SPM[p][:], start=True, stop=False),
                         reads=["TRI", kspm], writes=[ck], sig=False)
                    if not first:
                        T.op("pe", lambda e: e.matmul(cb[:, :], lhsT=ONES[:], rhs=R[p][:], start=False, stop=False),
                             reads=["ONES", kr], writes=[ck], sig=False)
                    T.op("pe", lambda e, ks=ks, qs=qs: e.matmul(cb[:, :], lhsT=KT[bs][:, ks], rhs=QT[bs][:, qs], start=False, stop=True),
                         reads=[kk, kq], writes=[ck])
                    if kb > 0:
                        if first:
                            T.op("dve", lambda e: e.tensor_copy(out=R[p][:], in_=SPM[p][:]), reads=[kspm], writes=[kr])
                        else:
                            T.op("dve", lambda e: e.tensor_tensor(out=R[p][:], in0=R[p][:], in1=SPM[p][:], op=ALU.add),
                                 reads=[kr, kspm], writes=[kr])
                    T.op("act", lambda e: e.activation(out=WW[p][:], in_=cb[:, :], func=AF.Exp), reads=[ck], writes=[kww])
                    if r >= 0:
                        T.op("dve", lambda e, msl=msl: e.tensor_tensor(out=WW[p][:], in0=WW[p][:], in1=MB[:, msl], op=ALU.mult),
                             reads=[kww, "MB"], writes=[kww])
                    T.op("pe", lambda e, kb=kb, first=first: e.matmul(ob[:, :], lhsT=V[bs][:, kb, :], rhs=WW[p][:], start=first, stop=(kb == 0)),
                         reads=[kv, kww], writes=[okey])
                T.op("dve", lambda e, qs=qs: e.tensor_copy(out=OT[:, hl, qs], in_=ob[:, :]), reads=[okey], writes=["OT"])

        npair = (nh + 1) // 2
        for hl in range(0, min(2, nh)):
            emit_proj(hl)
        for s_ in range(npair):
            streams = []
            for hl in range(2 * s_, min(2 * s_ + 2, nh)):
                T.begin_capture(); emit_att(hl); streams.append(T.end_capture())
            for hl in range(2 * s_ + 2, min(2 * s_ + 4, nh)):
                T.begin_capture(); emit_proj(hl); streams.append(T.end_capture())
            T.merge(streams)
        oi = 0
        for ncn in range(4):
            for h2 in range(HL):
                T.dma("pool", lambda e, h2=h2, ncn=ncn: e.dma_start(
                    out=WO[:, h2 * 512:(h2 + 1) * 512], in_=wo[:, h2 * D + ncn * 512:h2 * D + (ncn + 1) * 512]),
                    writes=["WO"], sem="WO%d" % (h2 % 2))
            for tt in range(16):
                bank = PS[oi % 4]
                bk = "ps%d" % (oi % 4)
                for hl in range(HL):
                    T.op("pe", lambda e, bank=bank, hl=hl, tt=tt: e.matmul(
                        bank[:, :], lhsT=OT[:, hl, tt * 128:(tt + 1) * 128], rhs=WO[:, hl * 512:(hl + 1) * 512],
                        start=(hl == 0), stop=(hl == HL - 1)), reads=["OT", "WO"], writes=[bk])
                os_ = OST[oi % 2]
                ko = "OST%d" % (oi % 2)
                T.op("act", lambda e, bank=bank, os_=os_: e.copy(out=os_[:], in_=bank[:, :]), reads=[bk], writes=[ko])
                T.dma("sp", lambda e, os_=os_, tt=tt, ncn=ncn: e.dma_start(
                    out=pout[tt * 128:(tt + 1) * 128, ncn * 512:(ncn + 1) * 512], in_=os_[:]),
                    reads=[ko], writes=["out"], sem="o" + ko)
                oi += 1
        T.streams["sp"].append(([(n, v) for n, v in T.dma_cnt.items() if n.startswith("d_oOST")], None, None, 0))
        T.emit(st)
    return nc


def att_consts():
    bf = ml_dtypes.bfloat16
    s = np.arange(128)[:, None]
    u = np.arange(896)[None, :]
    c = {}
    c["cmask"] = (u > s + 384).astype(np.float32).astype(bf)
    j = np.arange(128)[:, None]
    ss = np.arange(128)[None, :]
    c["ctri"] = (-(j >= ss).astype(np.float32)).astype(bf)
    return c


def att_inputs(xb, w_qkv, w_o, hh):
    d = {}
    d["xT"] = np.ascontiguousarray(xb.T).reshape(16, 128, SEQ)
    heads = slice(hh * HL, (hh + 1) * HL)
    w = w_qkv.reshape(16, 128, 3, 16, 128)[:, :, :, heads, :]
    d["wqkv"] = np.ascontiguousarray(w.transpose(3, 1, 2, 0, 4)).reshape(HL, 128, 3 * 16 * 128)
    w = w_o.reshape(16, 128, D_MODEL)[heads]
    d["wo"] = np.ascontiguousarray(w.transpose(1, 0, 2)).reshape(128, HL * D_MODEL)
    return d


def _prog_check(T):
    pos = {e: 0 for e in ENGS}
    sem = {}
    progress = True
    while progress:
        progress = False
        for e in ENGS:
            while pos[e] < len(T.streams[e]):
                waits, fn, s, inc = T.streams[e][pos[e]]
                if all(sem.get(ws, 0) >= wv for ws, wv in waits):
                    if s is not None:
                        sem[s] = sem.get(s, 0) + inc
                    pos[e] += 1
                    progress = True
                else:
                    break
    return {e: (pos[e], len(T.streams[e])) for e in ENGS}


GKH = 8
GVH = 16


def build_gdn(nkh=GKH, ntl=16, stage=99):
    nc = bass.Bass("TRN2", target_bir_lowering=False)
    D = D_MODEL
    dt = nc.dram_tensor
    xT = dt("xT", [16, 128, SEQ], F32, kind="ExternalInput").ap()
    win_qk = dt("win_qk", [GKH, 128, 2 * 16 * 128], F32, kind="ExternalInput").ap()
    win_vz = dt("win_vz", [GVH, 128, 2 * 16 * 128], F32, kind="ExternalInput").ap()
    wba = dt("wba", [128, 16 * 32], F32, kind="ExternalInput").ap()
    cw_qk = dt("cw_qk", [GKH, 128, 8], F32, kind="ExternalInput").ap()
    cw_v = dt("cw_v", [GVH, 128, 4], F32, kind="ExternalInput").ap()
    alog = dt("alog", [1, 16], F32, kind="ExternalInput").ap()
    dtb = dt("dtb", [1, 16], F32, kind="ExternalInput").ap()
    normw = dt("normw", [128, 1], F32, kind="ExternalInput").ap()
    wo = dt("wo", [128, GVH * D], F32, kind="ExternalInput").ap()
    cnames = ["identf", "tri2", "lastsel", "lsel0", "lsel1", "negm", "negmt", "strictm", "onesf"]
    cin = {n: dt("c_" + n, [128, 128], F32, kind="ExternalInput").ap() for n in cnames}
    c_sel16 = dt("c_sel16", [128, 16 * 128], F32, kind="ExternalInput").ap()
    c_cmcol = dt("c_cmcol", [128, 2], F32, kind="ExternalInput").ap()
    c_cmrow = dt("c_cmrow", [128, 256], F32, kind="ExternalInput").ap()
    ogs = dt("ogs", [16, 128, GVH * 128], BF16, kind="Internal").ap()
    pout = dt("pout", [SEQ, D], F32, kind="ExternalOutput").ap()

    with ExitStack() as st:
        def sb(name, shape, dtype=F32):
            return st.enter_context(nc.sbuf_tensor(name, shape, dtype))
        PS = [st.enter_context(nc.psum_tensor("ps%d" % i, [128, 512], F32)) for i in range(8)]
        T = Prog(nc)
        qn = [0]

        qd = [0]

        def QA():
            i = qn[0] % 8
            qn[0] += 1
            b = 4 + i // 4
            return PS[b][:, (i % 4) * 128:(i % 4 + 1) * 128], "ps%d" % b

        def QD():
            i = qd[0] % 8
            qd[0] += 1
            b = 6 + i // 4
            return PS[b][:, (i % 4) * 128:(i % 4 + 1) * 128], "ps%d" % b

        XT = sb("XT", [128, 16, SEQ], BF16)
        C = {n: sb("C_" + n, [128, 128]) for n in cnames}
        SEL16 = sb("SEL16", [128, 16 * 128])
        CMCOL = sb("CMCOL", [128, 2])
        CMROW = sb("CMROW", [128, 256])
        WBA = sb("WBA", [128, 16 * 32], BF16)
        NEGA = sb("NEGA", [128, 16])
        DTB = sb("DTB", [128, 16])
        NORMW = sb("NORMW", [128, 1])
        ONE1 = sb("ONE1", [128, 1])
        EPS6 = sb("EPS6", [128, 1])
        BETA = sb("BETA", [128, 16, 16])
        NBETA = sb("NBETA", [128, 16, 16])
        BK = sb("BK", [128, 16, 16])
        DCY0 = sb("DCY0", [128, 16, 16])
        DCY1 = sb("DCY1", [128, 16, 16])
        EGL0 = sb("EGL0", [128, 16, 16])
        EGL1 = sb("EGL1", [128, 16, 16])
        GCT = sb("GCT", [128, 2, 128])
        NGCT = sb("NGCT", [128, 2, 128])
        EGT0 = sb("EGT0", [128, 2, 128])
        EGT1 = sb("EGT1", [128, 2, 128])
        GP = sb("GP", [128, 128])
        GALL = sb("GALL", [128, 16, 16])
        g_t = sb("g_t", [128, 16])
        gc_t = sb("gc_t", [128, 16])
        tmp16 = sb("tmp16", [128, 16])
        tmpT = sb("tmpT", [128, 128])
        ba_t = sb("ba_t", [128, 32])
        l48 = sb("l48", [128, 48])

        for n in cnames:
            T.dma("sp", lambda e, n=n: e.dma_start(out=C[n][:], in_=cin[n]), writes=["C_" + n])
        T.dma("sp", lambda e: e.dma_start(out=SEL16[:], in_=c_sel16), writes=["SEL16"])
        T.dma("sp", lambda e: e.dma_start(out=CMCOL[:], in_=c_cmcol), writes=["CMCOL"])
        T.dma("sp", lambda e: e.dma_start(out=CMROW[:], in_=c_cmrow), writes=["CMROW"])
        T.dma("sp", lambda e: e.dma_start(out=NEGA[:], in_=alog.partition_broadcast(128)), writes=["NEGA"])
        T.dma("sp", lambda e: e.dma_start(out=DTB[:], in_=dtb.partition_broadcast(128)), writes=["DTB"])
        T.dma("sp", lambda e: e.dma_start(out=NORMW[:], in_=normw), writes=["NORMW"])
        T.dma("pool", lambda e: e.dma_start(out=WBA[:], in_=wba), writes=["WBA"])
        for fc in range(16):
            T.dma("pool", lambda e, fc=fc: e.dma_start(out=XT[:, fc, :], in_=xT[fc]), writes=["XT%d" % fc])
        T.op("dve", lambda e: e.memset(ONE1[:], 1.0), writes=["ONE1"])
        T.op("dve", lambda e: e.memset(GP[:], 0.0), writes=["GP"])
        T.op("dve", lambda e: e.memset(EPS6[:], 1e-6), writes=["EPS6"])
        T.op("act", lambda e: e.activation(out=NEGA[:], in_=NEGA[:], func=AF.Exp), reads=["NEGA"], writes=["NEGA"])
        T.op("dve", lambda e: e.tensor_scalar(out=NEGA[:], in0=NEGA[:], scalar1=-1.0, scalar2=None, op0=ALU.mult),
             reads=["NEGA"], writes=["NEGA"])

        gate_streams = []
        for ti in range(ntl):
            T.begin_capture()
            ts_ = slice(ti * 128, (ti + 1) * 128)
            pb_, kb_ = PS[6], "ps6"
            for fc in range(16):
                T.op("pe", lambda e, fc=fc, ts_=ts_: e.matmul(pb_[:, 0:32], lhsT=XT[:, fc, ts_], rhs=WBA[:, fc * 32:(fc + 1) * 32],
                                                              start=(fc == 0), stop=(fc == 15)), reads=["XT%d" % fc, "WBA"], writes=[kb_])
            T.op("dve", lambda e: e.tensor_copy(out=ba_t[:], in_=pb_[:, 0:32]), reads=[kb_], writes=["ba_t"])
            T.op("act", lambda e: e.activation(out=tmp16[:], in_=ba_t[:, 0:16], func=AF.Exp, scale=-1.0), reads=["ba_t"], writes=["tmp16"])
            T.op("dve", lambda e: e.tensor_scalar(out=tmp16[:], in0=tmp16[:], scalar1=1.0, scalar2=None, op0=ALU.add),
                 reads=["tmp16"], writes=["tmp16"])
            T.op("dve", lambda e, ti=ti: e.reciprocal(out=BETA[:, ti, :], in_=tmp16[:]), reads=["tmp16"], writes=["BETA"])
            T.op("dve", lambda e, ti=ti: e.tensor_scalar(out=NBETA[:, ti, :], in0=BETA[:, ti, :], scalar1=-1.0, scalar2=None, op0=ALU.mult),
                 reads=["BETA"], writes=["NBETA"])
            T.op("dve", lambda e: e.tensor_tensor(out=g_t[:], in0=ba_t[:, 16:32], in1=DTB[:], op=ALU.add), reads=["ba_t", "DTB"], writes=["g_t"])
            T.op("act", lambda e: e.activation(out=g_t[:], in_=g_t[:], func=AF.Exp), reads=["g_t"], writes=["g_t"])
            T.op("act", lambda e: e.activation(out=g_t[:], in_=g_t[:], func=AF.Ln, bias=ONE1[:, 0:1], scale=1.0),
                 reads=["g_t", "ONE1"], writes=["g_t"])
            T.op("dve", lambda e: e.tensor_tensor(out=g_t[:], in0=g_t[:], in1=NEGA[:], op=ALU.mult), reads=["g_t", "NEGA"], writes=["g_t"])
            T.op("pe", lambda e: e.matmul(PS[4][:, 0:16], lhsT=C["tri2"][:], rhs=g_t[:], start=True, stop=True),
                 reads=["C_tri2", "g_t"], writes=["ps4"])
            T.op("dve", lambda e, ti=ti: e.tensor_copy(out=GALL[:, ti, :], in_=g_t[:]), reads=["g_t"], writes=["GALL"])
            T.op("act", lambda e: e.copy(out=gc_t[:], in_=PS[4][:, 0:16]), reads=["ps4"], writes=["gc_t"])
            T.op("act", lambda e: e.activation(out=tmp16[:], in_=gc_t[:], func=AF.Exp), reads=["gc_t"], writes=["tmp16"])
            T.op("dve", lambda e, ti=ti: e.tensor_tensor(out=BK[:, ti, :], in0=tmp16[:], in1=BETA[:, ti, :], op=ALU.mult),
                 reads=["tmp16", "BETA"], writes=["BK"])
            T.op("pe", lambda e: e.matmul(PS[7][:, 0:16], lhsT=C["lastsel"][:], rhs=gc_t[:], start=True, stop=True),
                 reads=["C_lastsel", "gc_t"], writes=["ps7"])
            T.op("pe", lambda e: e.matmul(PS[7][:, 16:32], lhsT=C["lsel0"][:], rhs=gc_t[:], start=True, stop=True),
                 reads=["C_lsel0", "gc_t"], writes=["ps7"])
            T.op("pe", lambda e: e.matmul(PS[7][:, 32:48], lhsT=C["lsel1"][:], rhs=gc_t[:], start=True, stop=True),
                 reads=["C_lsel1", "gc_t"], writes=["ps7"])
            T.op("dve", lambda e: e.tensor_copy(out=l48[:], in_=PS[7][:, 0:48]), reads=["ps7"], writes=["l48"])
            T.op("dve", lambda e: e.tensor_tensor(out=tmp16[:], in0=l48[:, 0:16], in1=gc_t[:], op=ALU.subtract),
                 reads=["l48", "gc_t"], writes=["tmp16"])
            T.op("act", lambda e: e.activation(out=tmp16[:], in_=tmp16[:], func=AF.Exp), reads=["tmp16"], writes=["tmp16"])
            T.op("dve", lambda e, ti=ti: e.tensor_scalar(out=DCY0[:, ti, :], in0=tmp16[:], scalar1=CMCOL[:, 0:1], scalar2=None, op0=ALU.mult),
                 reads=["tmp16", "CMCOL"], writes=["DCY0"])
            T.op("dve", lambda e, ti=ti: e.tensor_scalar(out=DCY1[:, ti, :], in0=tmp16[:], scalar1=CMCOL[:, 1:2], scalar2=None, op0=ALU.mult),
                 reads=["tmp16", "CMCOL"], writes=["DCY1"])
            T.op("act", lambda e, ti=ti: e.activation(out=EGL0[:, ti, :], in_=l48[:, 16:32], func=AF.Exp), reads=["l48"], writes=["EGL0"])
            T.op("act", lambda e, ti=ti: e.activation(out=EGL1[:, ti, :], in_=l48[:, 32:48], func=AF.Exp), reads=["l48"], writes=["EGL1"])
            gate_streams.append(T.end_capture())

        WQK = sb("WQK", [128, 2 * 16 * 128], BF16)
        WVZ_ = sb("WVZ", [128, 2 * 16 * 128], BF16)
        WVZ = [WVZ_, WVZ_]
        CWQK = sb("CWQK", [128, 8])
        CWV = [sb("CWV%d" % j, [128, 4]) for j in range(2)]
        PQ = sb("PQ", [128, SEQ])
        CS = sb("CS", [128, SEQ])
        SQ = sb("SQ", [128, 512])
        QTn = sb("QTn", [128, SEQ], BF16)
        KTn = sb("KTn", [128, SEQ], BF16)
        KTM = sb("KTM", [128, 16, 128], BF16)
        VTM = [sb("VTM%d" % j, [128, 16, 128], BF16) for j in range(2)]
        ZS = [sb("ZS%d" % j, [128, SEQ], BF16) for j in range(2)]
        S = [sb("S%d" % j, [128, 128]) for j in range(2)]
        Sb0 = [sb("Sb0_%d" % j, [128, 128], BF16) for j in range(2)]
        Sb1 = [sb("Sb1_%d" % j, [128, 128], BF16) for j in range(2)]
        KKs = sb("KKs", [128, 2, 128])
        QKs = sb("QKs", [128, 2, 128])
        names32 = ["Dm", "DTm", "Q0", "P0", "Q1", "P1", "Y", "OTf", "OSQ", "RST"]
        t32 = [{n: sb("%s_%d" % (n, j), [128, 128]) for n in names32} for j in range(2)]
        names16 = ["KB", "VN0", "VN1", "VNM", "OG"]
        t16 = [{n: sb("%s_%d" % (n, j), [128, 128], BF16) for n in names16} for j in range(2)]

        IDB = sb("IDB", [128, 128], BF16)
        NEGB = {n: sb("NEGB_" + n, [128, 128], BF16) for n in ("negm", "negmt")}
        T.op("act", lambda e: e.copy(out=IDB[:], in_=C["identf"][:]), reads=["C_identf"], writes=["IDB"])
        for n in ("negm", "negmt"):
            T.op("act", lambda e, n=n: e.copy(out=NEGB[n][:], in_=C[n][:]), reads=["C_" + n], writes=["NEGB"])

        def mkpool(banks):
            st_ = [0]

            def alloc():
                i = st_[0] % (4 * len(banks))
                st_[0] += 1
                bnk = banks[i // 4]
                return PS[bnk][:, (i % 4) * 128:(i % 4 + 1) * 128], "ps%d" % bnk
            return alloc

        PPact = mkpool([3])
        PAact = [mkpool([4]), mkpool([5])]
        PAdve = [mkpool([6]), mkpool([7])]
        PBdve = [mkpool([0]), mkpool([2])]
        ifn = ["TTb", "VB", "NWT", "KD0", "KD1", "QE0", "QE1", "QKD"]
        IF = [[{n: sb("%s_%d_%d" % (n, j, p), [128, 128], BF16) for n in ifn} for p in range(2)] for j in range(2)]

        PQ2 = [PQ, sb("PQ1", [128, SEQ])]

        def conv_silu_chunk(cw, koff, ckey, pq):
            src, ksrc = PQ2[pq], "PQ%d" % pq
            T.op("dve", lambda e: e.tensor_scalar(out=CS[:], in0=src[:], scalar1=cw[:, koff + 3:koff + 4], scalar2=None, op0=ALU.mult),
                 reads=[ksrc, ckey], writes=["CS"])
            for sft in (1, 2, 3):
                T.op("dve", lambda e, sft=sft: e.scalar_tensor_tensor(
                    out=CS[:, sft:], in0=src[:, 0:SEQ - sft], scalar=cw[:, koff + 3 - sft:koff + 4 - sft], in1=CS[:, sft:],
                    op0=ALU.mult, op1=ALU.add), reads=[ksrc, ckey, "CS"], writes=["CS"])
            T.op("act", lambda e: e.activation(out=CS[:], in_=CS[:], func=AF.Silu), reads=["CS"], writes=["CS"])

        def project(w, woff, kw, pq):
            dst, kdst = PQ2[pq], "PQ%d" % pq
            for tcn in range(4):
                bank, bk = PS[tcn % 2], "ps%d" % (tcn % 2)
                for fc in range(16):
                    off = woff + fc * 128
                    T.op("pe", lambda e, bank=bank, fc=fc, off=off, tcn=tcn: e.matmul(
                        bank[:, :], lhsT=w[:, off:off + 128], rhs=XT[:, fc, tcn * 512:(tcn + 1) * 512],
                        start=(fc == 0), stop=(fc == 15)), reads=[kw, "XT%d" % fc], writes=[bk], sig=(fc == 15))
                T.op("act", lambda e, bank=bank, tcn=tcn: e.copy(out=dst[:, tcn * 512:(tcn + 1) * 512], in_=bank[:, :]),
                     reads=[bk], writes=[kdst])

        def l2norm_to(dst, kdst, scale, keep=False):
            for tcn in range(4):
                cs_ = slice(tcn * 512, (tcn + 1) * 512)
                bank, bk = PS[2 + tcn % 2], "ps%d" % (2 + tcn % 2)
                T.op("dve", lambda e, cs_=cs_: e.tensor_tensor(out=SQ[:], in0=CS[:, cs_], in1=CS[:, cs_], op=ALU.mult), reads=["CS"], writes=["SQ"])
                T.op("pe", lambda e, bank=bank: e.matmul(bank[:, :], lhsT=C["onesf"][:], rhs=SQ[:], start=True, stop=True),
                     reads=["C_onesf", "SQ"], writes=[bk])
                T.op("act", lambda e, bank=bank: e.activation(out=SQ[:], in_=bank[:, :], func=AF.Ln, bias=EPS6[:, 0:1], scale=1.0),
                     reads=[bk, "EPS6"], writes=["SQ"])
                T.op("act", lambda e: e.activation(out=SQ[:], in_=SQ[:], func=AF.Exp, scale=-0.5), reads=["SQ"], writes=["SQ"])
                T.op("dve", lambda e, cs_=cs_: e.scalar_tensor_tensor(out=dst[:, cs_], in0=CS[:, cs_], scalar=scale, in1=SQ[:], op0=ALU.mult, op1=ALU.mult),
                     reads=["CS", "SQ"], writes=[kdst])
                if keep:
                    T.op("dve", lambda e, cs_=cs_: e.tensor_tensor(out=CS[:, cs_], in0=CS[:, cs_], in1=SQ[:], op=ALU.mult), reads=["CS", "SQ"], writes=["CS"])

        def transposes_to(dst, kdst):
            for q4 in range(4):
                bank, bk = PS[2 + q4 % 2], "ps%d" % (2 + q4 % 2)
                for j4 in range(4):
                    tt = q4 * 4 + j4
                    T.op("pe", lambda e, bank=bank, j4=j4, tt=tt: e.transpose(
                        out=bank[:, j4 * 128:(j4 + 1) * 128], in_=CS[:, tt * 128:(tt + 1) * 128], identity=C["identf"][:]),
                        reads=["CS", "C_identf"], writes=[bk])
                T.op("act", lambda e, bank=bank, q4=q4: e.copy(out=dst[:, 4 * q4:4 * q4 + 4, :], in_=bank[:, :].rearrange("p (a b) -> p a b", a=4)),
                     reads=[bk], writes=[kdst])

        if nkh == 0:
            T.merge(gate_streams)
        for kh in range(nkh):
            def Pj(c, kh=kh):
                if c == 0:
                    T.dma("pool", lambda e: e.dma_start(out=WQK[:], in_=win_qk[kh]), writes=["WQK"])
                    T.dma("sp", lambda e: e.dma_start(out=CWQK[:], in_=cw_qk[kh]), writes=["CWQK"])
                    project(WQK, 0, "WQK", 0)
                elif c == 1:
                    project(WQK, 16 * 128, "WQK", 1)
                else:
                    j = (c - 2) // 2
                    if c % 2 == 0:
                        T.dma("pool", lambda e: e.dma_start(out=WVZ[j][:], in_=win_vz[2 * kh + j]), writes=["WVZ"])
                        T.dma("sp", lambda e: e.dma_start(out=CWV[j][:], in_=cw_v[2 * kh + j]), writes=["CWV%d" % j])
                        project(WVZ[j], 0, "WVZ", 0)
                    else:
                        project(WVZ[j], 16 * 128, "WVZ", 1)

            def Post(c):
                if c == 0:
                    conv_silu_chunk(CWQK, 0, "CWQK", 0)
                    l2norm_to(QTn, "QTn", float(128 ** -0.5))
                elif c == 1:
                    conv_silu_chunk(CWQK, 4, "CWQK", 1)
                    l2norm_to(KTn, "KTn", 1.0, keep=True)
                    transposes_to(KTM, "KTM")
                else:
                    j = (c - 2) // 2
                    if c % 2 == 0:
                        conv_silu_chunk(CWV[j], 0, "CWV%d" % j, 0)
                        transposes_to(VTM[j], "VTM%d" % j)
                    else:
                        T.op("act", lambda e: e.activation(out=ZS[j][:], in_=PQ2[1][:], func=AF.Silu), reads=["PQ1"], writes=["ZS%d" % j])
                        T.op("dve", lambda e: e.memset(S[j][:], 0.0), writes=["S%d" % j])
                        T.op("dve", lambda e: e.memset(Sb0[j][:], 0.0), writes=["Sb0_%d" % j])

            Pj(0)
            for c in range(6):
                streams = []
                if c + 1 < 6:
                    T.begin_capture(); Pj(c + 1); streams.append(T.end_capture())
                T.begin_capture(); Post(c); streams.append(T.end_capture())
                if kh == 0:
                    lo, hi = (c * ntl) // 6, ((c + 1) * ntl) // 6
                    gs = [op_ for tl_ in gate_streams[lo:hi] for op_ in tl_]
                    streams.append(gs)
                T.merge(streams)

            def emitP(ti):
                ts_ = slice(ti * 128, (ti + 1) * 128)
                pp = ti % 2
                kp = lambda n: "%s_p%d" % (n, pp)
                T.op("dve", lambda e: e.tensor_copy(out=GP[:, 0:16], in_=GALL[:, ti, :]), reads=["GALL"], writes=["GP"])
                T.op("pe", lambda e: e.matmul(PS[1][:, 128:256], lhsT=GP[:], rhs=C["tri2"][:], start=True, stop=True),
                     reads=["C_tri2", "GP"], writes=["ps1"])
                T.op("act", lambda e: e.copy(out=GCT[:, pp, :], in_=PS[1][:, 128:256]), reads=["ps1"], writes=[kp("GCT")])
                T.op("dve", lambda e: e.tensor_scalar(out=NGCT[:, pp, :], in0=GCT[:, pp, :], scalar1=-1.0, scalar2=None, op0=ALU.mult),
                     reads=[kp("GCT")], writes=[kp("NGCT")])
                T.op("act", lambda e: e.activation(out=tmpT[:], in_=GCT[:, pp, :], func=AF.Exp), reads=[kp("GCT")], writes=["tmpT"])
                T.op("dve", lambda e: e.tensor_tensor(out=EGT0[:, pp, :], in0=tmpT[:], in1=CMROW[:, 0:128], op=ALU.mult),
                     reads=["tmpT", "CMROW"], writes=[kp("EGT0")])
                T.op("dve", lambda e: e.tensor_tensor(out=EGT1[:, pp, :], in0=tmpT[:], in1=CMROW[:, 128:256], op=ALU.mult),
                     reads=["tmpT", "CMROW"], writes=[kp("EGT1")])
                a, ka = PPact()
                T.op("pe", lambda e, a=a: e.matmul(a, lhsT=KTn[:, ts_], rhs=KTn[:, ts_], start=True, stop=True),
                     reads=["KTn"], writes=[ka])
                T.op("act", lambda e, a=a: e.copy(out=KKs[:, pp, :], in_=a), reads=[ka], writes=[kp("KKs")])
                a, ka = PPact()
                T.op("pe", lambda e, a=a: e.matmul(a, lhsT=KTn[:, ts_], rhs=QTn[:, ts_], start=True, stop=True),
                     reads=["KTn", "QTn"], writes=[ka])
                T.op("act", lambda e, a=a: e.copy(out=QKs[:, pp, :], in_=a), reads=[ka], writes=[kp("QKs")])

            def emitA(ti, j):
                ts_ = slice(ti * 128, (ti + 1) * 128)
                h = 2 * kh + j
                pp = ti % 2
                kp = lambda n: "%s_p%d" % (n, pp)
                f, b = t32[j], IF[j][ti % 2]
                kb_t = t16[j]["KB"]
                K32 = lambda n: "%s_%d" % (n, j)
                KI = lambda n: "%s_%d_%d" % (n, j, ti % 2)
                QAa, QAd = PAact[j], PAdve[j]
                selh = SEL16[:, h * 128:(h + 1) * 128]
                a, ka = QAa()
                T.op("pe", lambda e, a=a: e.matmul(a, lhsT=GCT[:, pp, :], rhs=selh, start=True, stop=False), reads=[kp("GCT"), "SEL16"], writes=[ka], sig=False)
                T.op("pe", lambda e, a=a: e.matmul(a, lhsT=selh, rhs=NGCT[:, pp, :], start=False, stop=False), reads=[kp("NGCT"), "SEL16"], writes=[ka], sig=False)
                T.op("pe", lambda e, a=a: e.matmul(a, lhsT=IDB[:], rhs=NEGB["negm"][:], start=False, stop=True),
                     reads=["IDB", "NEGB"], writes=[ka])
                T.op("act", lambda e, a=a: e.activation(out=f["Dm"][:], in_=a, func=AF.Exp), reads=[ka], writes=[K32("Dm")])
                a, ka = QAa()
                T.op("pe", lambda e, a=a: e.matmul(a, lhsT=selh, rhs=GCT[:, pp, :], start=True, stop=False), reads=[kp("GCT"), "SEL16"], writes=[ka], sig=False)
                T.op("pe", lambda e, a=a: e.matmul(a, lhsT=NGCT[:, pp, :], rhs=selh, start=False, stop=False), reads=[kp("NGCT"), "SEL16"], writes=[ka], sig=False)
                T.op("pe", lambda e, a=a: e.matmul(a, lhsT=IDB[:], rhs=NEGB["negmt"][:], start=False, stop=True),
                     reads=["IDB", "NEGB"], writes=[ka])
                T.op("act", lambda e, a=a: e.activation(out=f["DTm"][:], in_=a, func=AF.Exp), reads=[ka], writes=[K32("DTm")])
                T.op("dve", lambda e: e.scalar_tensor_tensor(out=f["Q0"][:], in0=KKs[:, pp, :], scalar=NBETA[:, ti, h:h + 1], in1=f["Dm"][:],
                                                             op0=ALU.mult, op1=ALU.mult), reads=[kp("KKs"), "NBETA", K32("Dm")], writes=[K32("Q0")])
                T.op("dve", lambda e: e.tensor_tensor(out=f["Q0"][:], in0=f["Q0"][:], in1=C["strictm"][:], op=ALU.mult),
                     reads=[K32("Q0"), "C_strictm"], writes=[K32("Q0")])
                a, ka = QAa()
                T.op("pe", lambda e, a=a: e.transpose(out=a, in_=f["Q0"][:], identity=C["identf"][:]), reads=[K32("Q0"), "C_identf"], writes=[ka])
                T.op("act", lambda e, a=a: e.copy(out=f["P0"][:], in_=a), reads=[ka], writes=[K32("P0")])
                T.op("dve", lambda e: e.tensor_tensor(out=f["Y"][:], in0=f["P0"][:], in1=C["identf"][:], op=ALU.add),
                     reads=[K32("P0"), "C_identf"], writes=[K32("Y")])
                pq = [("P0", "Q0"), ("P1", "Q1")]
                for k in range(1, 6):
                    pprev, qprev = pq[(k - 1) % 2]
                    pcur, qcur = pq[k % 2]
                    if k <= 4:
                        a1, ka1 = QAa()
                        T.op("pe", lambda e, a1=a1, pprev=pprev, qprev=qprev: e.matmul(a1, lhsT=f[qprev][:], rhs=f[pprev][:], start=True, stop=True),
                             reads=[K32(pprev), K32(qprev)], writes=[ka1])
                    a2, ka2 = QAd()
                    T.op("pe", lambda e, a2=a2, pprev=pprev, qprev=qprev: e.matmul(a2, lhsT=f[pprev][:], rhs=f[qprev][:], start=True, stop=True),
                         reads=[K32(pprev), K32(qprev)], writes=[ka2])
                    if k <= 4:
                        T.op("act", lambda e, a1=a1, pcur=pcur: e.copy(out=f[pcur][:], in_=a1), reads=[ka1], writes=[K32(pcur)])
                    T.op("dve", lambda e, a2=a2, qcur=qcur: e.tensor_copy(out=f[qcur][:], in_=a2), reads=[ka2], writes=[K32(qcur)])
                    a3, ka3 = QAd()
                    T.op("pe", lambda e, a3=a3, qcur=qcur: e.matmul(a3, lhsT=f[qcur][:], rhs=f["Y"][:], start=True, stop=True),
                         reads=[K32(qcur), K32("Y")], writes=[ka3])
                    T.op("dve", lambda e, a3=a3: e.tensor_tensor(out=f["Y"][:], in0=f["Y"][:], in1=a3, op=ALU.add),
                         reads=[ka3, K32("Y")], writes=[K32("Y")])
                T.op("act", lambda e: e.copy(out=b["TTb"][:], in_=f["Y"][:]), reads=[K32("Y")], writes=[KI("TTb")])
                T.op("dve", lambda e: e.tensor_scalar(out=b["VB"][:], in0=VTM[j][:, ti, :], scalar1=BETA[:, ti, h:h + 1], scalar2=None, op0=ALU.mult),
                     reads=["VTM%d" % j, "BETA"], writes=[KI("VB")])
                T.op("dve", lambda e: e.tensor_scalar(out=kb_t[:], in0=KTM[:, ti, :], scalar1=BK[:, ti, h:h + 1], scalar2=None, op0=ALU.mult),
                     reads=["KTM", "BK"], writes=[K32("KB")])
                T.op("dve", lambda e: e.tensor_scalar(out=b["KD0"][:], in0=KTM[:, ti, :], scalar1=DCY0[:, ti, h:h + 1], scalar2=None, op0=ALU.mult),
                     reads=["KTM", "DCY0"], writes=[KI("KD0")])
                T.op("dve", lambda e: e.tensor_scalar(out=b["KD1"][:], in0=KTM[:, ti, :], scalar1=DCY1[:, ti, h:h + 1], scalar2=None, op0=ALU.mult),
                     reads=["KTM", "DCY1"], writes=[KI("KD1")])
                T.op("dve", lambda e: e.tensor_tensor(out=b["QKD"][:], in0=QKs[:, pp, :], in1=f["DTm"][:], op=ALU.mult),
                     reads=[kp("QKs"), K32("DTm")], writes=[KI("QKD")])
                for cidx, egt, qe in ((0, EGT0, "QE0"), (1, EGT1, "QE1")):
                    a, ka = QAd()
                    T.op("pe", lambda e, a=a, egt=egt: e.matmul(a, lhsT=selh, rhs=egt[:, pp, :], start=True, stop=True),
                         reads=["SEL16", kp("EGT%d" % cidx)], writes=[ka])
                    T.op("dve", lambda e, a=a, qe=qe: e.tensor_tensor(out=b[qe][:], in0=QTn[:, ts_], in1=a, op=ALU.mult),
                         reads=[ka, "QTn"], writes=[KI(qe)])
                a, ka = QAd()
                T.op("pe", lambda e, a=a: e.matmul(a, lhsT=kb_t[:], rhs=b["TTb"][:], start=True, stop=True),
                     reads=[K32("KB"), KI("TTb")], writes=[ka])
                T.op("dve", lambda e, a=a: e.tensor_scalar(out=b["NWT"][:], in0=a, scalar1=-1.0, scalar2=None, op0=ALU.mult),
                     reads=[ka], writes=[KI("NWT")])

            def emitB(ti, j):
                ts_ = slice(ti * 128, (ti + 1) * 128)
                h = 2 * kh + j
                f, b, w = t32[j], IF[j][ti % 2], t16[j]
                K32 = lambda n: "%s_%d" % (n, j)
                KI = lambda n: "%s_%d_%d" % (n, j, ti % 2)
                QB = PBdve[j]
                kS, kS0, kS1 = "S%d" % j, "Sb0_%d" % j, "Sb1_%d" % j
                a, ka = QB()
                T.op("pe", lambda e, a=a: e.matmul(a, lhsT=b["TTb"][:], rhs=b["VB"][:], start=True, stop=False), reads=[KI("TTb"), KI("VB")], writes=[ka])
                T.op("pe", lambda e, a=a: e.matmul(a, lhsT=b["NWT"][:], rhs=Sb0[j][:], start=False, stop=True), reads=[KI("NWT"), kS0], writes=[ka])
                T.op("dve", lambda e, a=a: e.tensor_copy(out=w["VN0"][:], in_=a), reads=[ka], writes=[K32("VN0")])
                a, ka = QB()
                T.op("pe", lambda e, a=a: e.matmul(a, lhsT=b["KD0"][:], rhs=w["VN0"][:], start=True, stop=True), reads=[KI("KD0"), K32("VN0")], writes=[ka])
                T.op("dve", lambda e, a=a: e.scalar_tensor_tensor(out=S[j][:], in0=S[j][:], scalar=EGL0[:, ti, h:h + 1], in1=a,
                                                                  op0=ALU.mult, op1=ALU.add), reads=[kS, "EGL0", ka], writes=[kS])
                T.op("act", lambda e: e.copy(out=Sb1[j][:], in_=S[j][:]), reads=[kS], writes=[kS1])
                a, ka = QB()
                T.op("pe", lambda e, a=a: e.matmul(a, lhsT=b["TTb"][:], rhs=b["VB"][:], start=True, stop=False), reads=[KI("TTb"), KI("VB")], writes=[ka])
                T.op("pe", lambda e, a=a: e.matmul(a, lhsT=b["NWT"][:], rhs=Sb1[j][:], start=False, stop=True), reads=[KI("NWT"), kS1], writes=[ka])
                T.op("dve", lambda e, a=a: e.tensor_copy(out=w["VN1"][:], in_=a), reads=[ka], writes=[K32("VN1")])
                T.op("dve", lambda e: e.tensor_scalar(out=w["VNM"][:], in0=w["VN0"][:], scalar1=CMCOL[:, 0:1], scalar2=None, op0=ALU.mult),
                     reads=[K32("VN0"), "CMCOL"], writes=[K32("VNM")])
                T.op("dve", lambda e: e.scalar_tensor_tensor(out=w["VNM"][:], in0=w["VN1"][:], scalar=CMCOL[:, 1:2], in1=w["VNM"][:],
                                                             op0=ALU.mult, op1=ALU.add), reads=[K32("VN1"), "CMCOL", K32("VNM")], writes=[K32("VNM")])
                a, ka = QB()
                T.op("pe", lambda e, a=a: e.matmul(a, lhsT=Sb0[j][:], rhs=b["QE0"][:], start=True, stop=False), reads=[kS0, KI("QE0")], writes=[ka])
                T.op("pe", lambda e, a=a: e.matmul(a, lhsT=Sb1[j][:], rhs=b["QE1"][:], start=False, stop=False), reads=[kS1, KI("QE1")], writes=[ka])
                T.op("pe", lambda e, a=a: e.matmul(a, lhsT=w["VNM"][:], rhs=b["QKD"][:], start=False, stop=True), reads=[K32("VNM"), KI("QKD")], writes=[ka])
                T.op("dve", lambda e, a=a: e.tensor_copy(out=f["OTf"][:], in_=a), reads=[ka], writes=[K32("OTf")])
                T.op("dve", lambda e: e.tensor_tensor(out=f["OSQ"][:], in0=f["OTf"][:], in1=f["OTf"][:], op=ALU.mult),
                     reads=[K32("OTf")], writes=[K32("OSQ")])
                a, ka = QB()
                T.op("pe", lambda e, a=a: e.matmul(a, lhsT=b["KD1"][:], rhs=w["VN1"][:], start=True, stop=True), reads=[KI("KD1"), K32("VN1")], writes=[ka])
                T.op("dve", lambda e, a=a: e.scalar_tensor_tensor(out=S[j][:], in0=S[j][:], scalar=EGL1[:, ti, h:h + 1], in1=a,
                                                                  op0=ALU.mult, op1=ALU.add), reads=[kS, "EGL1", ka], writes=[kS])
                T.op("act", lambda e: e.copy(out=Sb0[j][:], in_=S[j][:]), reads=[kS], writes=[kS0])
                a, ka = QB()
                T.op("pe", lambda e, a=a: e.matmul(a, lhsT=C["onesf"][:], rhs=f["OSQ"][:], start=True, stop=True), reads=["C_onesf", K32("OSQ")], writes=[ka])
                T.op("dve", lambda e, a=a: e.tensor_scalar(out=f["RST"][:], in0=a, scalar1=1.0 / 128, scalar2=1e-6, op0=ALU.mult, op1=ALU.add),
                     reads=[ka], writes=[K32("RST")])
                T.op("act", lambda e: e.activation(out=f["RST"][:], in_=f["RST"][:], func=AF.Ln), reads=[K32("RST")], writes=[K32("RST")])
                T.op("act", lambda e: e.activation(out=f["RST"][:], in_=f["RST"][:], func=AF.Exp, scale=-0.5), reads=[K32("RST")], writes=[K32("RST")])
                T.op("dve", lambda e: e.scalar_tensor_tensor(out=f["OTf"][:], in0=f["OTf"][:], scalar=NORMW[:, 0:1], in1=f["RST"][:],
                                                             op0=ALU.mult, op1=ALU.mult), reads=[K32("OTf"), "NORMW", K32("RST")], writes=[K32("OTf")])
                T.op("dve", lambda e: e.tensor_tensor(out=w["OG"][:], in0=f["OTf"][:], in1=ZS[j][:, ts_], op=ALU.mult),
                     reads=[K32("OTf"), "ZS%d" % j], writes=[K32("OG")])
                T.dma("sp", lambda e: e.dma_start(out=ogs[ti, :, h * 128:(h + 1) * 128], in_=w["OG"][:]),
                      reads=[K32("OG")], writes=["ogs%d" % j], sem="ogs%d" % j)

            if ntl > 0:
                emitP(0)
            for step in range(ntl + 1):
                streams = []
                if step + 1 < ntl:
                    T.begin_capture(); emitP(step + 1); streams.append(T.end_capture())
                if step < ntl:
                    for j in range(2):
                        T.begin_capture(); emitA(step, j); streams.append(T.end_capture())
                if step >= 1:
                    for j in range(2):
                        T.begin_capture(); emitB(step - 1, j); streams.append(T.end_capture())
                T.merge(streams)

        T.mute = False
        WO = sb("WO", [128, GVH * 512], BF16)
        OGT_ = sb("OGT", [128, GVH * 128], BF16)
        OGT = [OGT_, OGT_]
        OST = [CS[:, 0:512], CS[:, 512:1024]]
        oi = 0
        for ncn in range(4):
            for h in range(GVH):
                T.dma("pool", lambda e, h=h, ncn=ncn: e.dma_start(
                    out=WO[:, h * 512:(h + 1) * 512], in_=wo[:, h * D + ncn * 512:h * D + (ncn + 1) * 512]),
                    writes=["WO"], sem="WO%d" % (h % 2))
            for tt in range(16):
                og = OGT[oi % 2]
                kog = "OGT"
                T.dma("sp", lambda e, og=og, tt=tt: e.dma_start(out=og[:], in_=ogs[tt]), reads=["ogs0", "ogs1"], writes=[kog])
                bank, bk = PS[oi % 4], "ps%d" % (oi % 4)
                for h in range(GVH):
                    T.op("pe", lambda e, bank=bank, og=og, h=h: e.matmul(
                        bank[:, :], lhsT=og[:, h * 128:(h + 1) * 128], rhs=WO[:, h * 512:(h + 1) * 512],
                        start=(h == 0), stop=(h == GVH - 1)), reads=[kog, "WO"], writes=[bk])
                os_ = OST[oi % 2]
                ko = "OST%d" % (oi % 2)
                T.op("act", lambda e, bank=bank, os_=os_: e.copy(out=os_, in_=bank[:, :]), reads=[bk, "CS"], writes=[ko])
                T.dma("sp", lambda e, os_=os_, tt=tt, ncn=ncn: e.dma_start(
                    out=pout[tt * 128:(tt + 1) * 128, ncn * 512:(ncn + 1) * 512], in_=os_),
                    reads=[ko], writes=["out"], sem="o" + ko)
                oi += 1
        T.streams["sp"].append(([(n, v) for n, v in T.dma_cnt.items() if n.startswith("d_oOST")], None, None, 0))
        T.emit(st)
    return nc


def gdn_consts():
    c = {}
    t = np.arange(128)
    same = (t[:, None] // 64) == (t[None, :] // 64)
    f = np.float32
    c["c_identf"] = np.eye(128, dtype=f)
    c["c_tri2"] = (same & (t[:, None] <= t[None, :])).astype(f)
    last = (t // 64) * 64 + 63
    c["c_lastsel"] = (t[:, None] == last[None, :]).astype(f)
    c["c_lsel0"] = np.tile((t == 63).astype(f)[:, None], (1, 128))
    c["c_lsel1"] = np.tile((t == 127).astype(f)[:, None], (1, 128))
    low = same & (t[:, None] >= t[None, :])
    c["c_negm"] = np.where(low, 0.0, -30000.0).astype(f)
    c["c_negmt"] = np.ascontiguousarray(c["c_negm"].T)
    c["c_strictm"] = (same & (t[:, None] > t[None, :])).astype(f)
    c["c_onesf"] = np.ones((128, 128), f)
    sel = np.zeros((128, 16, 128), f)
    for h in range(16):
        sel[h, h, :] = 1.0
    c["c_sel16"] = sel.reshape(128, 16 * 128)
    c["c_cmcol"] = np.stack([(t < 64), (t >= 64)], 1).astype(f)
    cm = np.zeros((128, 256), f)
    cm[0:16] = np.concatenate([(t < 64), (t >= 64)]).astype(f)[None, :]
    c["c_cmrow"] = cm
    return c


def gdn_inputs(xb, w_in, conv_w, a_log, dt_bias, norm_w, w_o, hh):
    d = {}
    d["xT"] = np.ascontiguousarray(xb.T).reshape(16, 128, SEQ)
    W = w_in.reshape(16, 128, -1)
    def cols(base, n):
        return W[:, :, base:base + n]
    q = cols(0 + hh * 1024, 1024).reshape(16, 128, 8, 128)
    k = cols(2048 + hh * 1024, 1024).reshape(16, 128, 8, 128)
    qk = np.stack([q, k], 0)
    d["win_qk"] = np.ascontiguousarray(qk.transpose(3, 2, 0, 1, 4)).reshape(8, 128, 2 * 16 * 128)
    v = cols(4096 + hh * 2048, 2048).reshape(16, 128, 16, 128)
    z = cols(8192 + hh * 2048, 2048).reshape(16, 128, 16, 128)
    vz = np.stack([v, z], 0)
    d["win_vz"] = np.ascontiguousarray(vz.transpose(3, 2, 0, 1, 4)).reshape(16, 128, 2 * 16 * 128)
    bcol = cols(12288 + hh * 16, 16)
    acol = cols(12320 + hh * 16, 16)
    d["wba"] = np.ascontiguousarray(np.concatenate([bcol, acol], 2).transpose(1, 0, 2)).reshape(128, 16 * 32)
    cq = conv_w[:, hh * 1024:(hh + 1) * 1024].reshape(4, 8, 128)
    ck = conv_w[:, 2048 + hh * 1024:2048 + (hh + 1) * 1024].reshape(4, 8, 128)
    d["cw_qk"] = np.ascontiguousarray(np.stack([cq, ck], 0).transpose(2, 3, 0, 1)).reshape(8, 128, 8)
    cv = conv_w[:, 4096 + hh * 2048:4096 + (hh + 1) * 2048].reshape(4, 16, 128)
    d["cw_v"] = np.ascontiguousarray(cv.transpose(1, 2, 0))
    d["alog"] = np.ascontiguousarray(a_log[hh * 16:(hh + 1) * 16].reshape(1, 16))
    d["dtb"] = np.ascontiguousarray(dt_bias[hh * 16:(hh + 1) * 16].reshape(1, 16))
    d["normw"] = np.ascontiguousarray(norm_w.reshape(128, 1))
    w = w_o.reshape(32, 128, D_MODEL)[hh * 16:(hh + 1) * 16]
    d["wo"] = np.ascontiguousarray(w.transpose(1, 0, 2)).reshape(128, 16 * D_MODEL)
    return d


def _run(nc, ims):
    return run_bass_kernel_spmd(nc, ims, core_ids=list(range(8))).results


def kernel(x, sb_w_qkv, sb_w_o, gdn_w_in, gdn_conv_w, gdn_a_log, gdn_dt_bias, gdn_norm_w, gdn_w_o,
           ln_mix_g, ln_mix_b, ln_ffn_g, ln_ffn_b, moe_router_w, moe_router_b,
           moe_w_gu, moe_b_gu, moe_w_down, moe_b_down):
    f = lambda a: np.ascontiguousarray(np.asarray(a, dtype=np.float32))
    x = f(x)
    B = x.shape[0]
    rows = lambda c: slice((c % 2) * 1024, (c % 2 + 1) * 1024)
    mc = moe_consts()
    progs = (build_moe_r(), build_moe_x(), build_moe_c())

    ac = att_consts()
    ims = []
    for c in range(8):
        im = att_inputs(x[c // 2], f(sb_w_qkv[0]), f(sb_w_o[0]), c % 2)
        im.update(ac)
        ims.append(im)
    ra = _run(build_att(), ims)
    xin_l = [x[c // 2][rows(c)] for c in range(8)]
    p0_l = [ra[2 * (c // 2)]["pout"][rows(c)] for c in range(8)]
    p1_l = [ra[2 * (c // 2) + 1]["pout"][rows(c)] for c in range(8)]
    rd, wper = moe_weights(f(moe_router_w[0]), f(moe_router_b[0]), f(moe_w_gu[0]), f(moe_b_gu[0]),
                           f(moe_w_down[0]), f(moe_b_down[0]))
    x2 = run_moe_layer(progs, xin_l, p0_l, p1_l,
                       np.stack([f(ln_mix_g[0]), f(ln_mix_b[0])]), np.stack([f(ln_ffn_g[0]), f(ln_ffn_b[0])]),
                       rd, wper, mc)
    del wper

    gcn = gdn_consts()
    ims = []
    for c in range(8):
        b = c // 2
        xb = np.concatenate([x2[2 * b], x2[2 * b + 1]], axis=0)
        im = gdn_inputs(xb, f(gdn_w_in[0]), f(gdn_conv_w[0]), f(gdn_a_log[0]), f(gdn_dt_bias[0]),
                        f(gdn_norm_w[0]), f(gdn_w_o[0]), c % 2)
        im.update(gcn)
        ims.append(im)
    rg = _run(build_gdn(), ims)
    p0_l = [rg[2 * (c // 2)]["pout"][rows(c)] for c in range(8)]
    p1_l = [rg[2 * (c // 2) + 1]["pout"][rows(c)] for c in range(8)]
    rd, wper = moe_weights(f(moe_router_w[1]), f(moe_router_b[1]), f(moe_w_gu[1]), f(moe_b_gu[1]),
                           f(moe_w_down[1]), f(moe_b_down[1]))
    x4 = run_moe_layer(progs, x2, p0_l, p1_l,
                       np.stack([f(ln_mix_g[1]), f(ln_mix_b[1])]), np.stack([f(ln_ffn_g[1]), f(ln_ffn_b[1])]),
                       rd, wper, mc)
    out = np.stack([np.concatenate([x4[2 * b], x4[2 * b + 1]], axis=0) for b in range(B)], axis=0)
    return out.astype(np.float32)
```
